# Optimizing a Trainium2 kernel written in Bass

```python
import jax, jax.numpy as jnp
from jax import lax
import numpy as np

D_MODEL = 1024
BATCH = 32
SEQ = 2048
DEPTH = 4

RNN_WIDTH = 1024
RNN_BLOCKS = 16
RNN_CONV = 4
LRU_C = 8.0
NSA_HEADS = 16
NSA_KV_HEADS = 4
HEAD_DIM = 64
GQA_REP = NSA_HEADS // NSA_KV_HEADS
CMP_BLOCK = 32
CMP_STRIDE = 16
CMP_HIDDEN = 128
SLC_BLOCK = 64
SLC_TOPK = 4
WINDOW = 256
Q_CHUNK = 64
FORCE_BONUS = 1.0e4
ROPE_THETA = 500000.0
ROPE_DIM = HEAD_DIM // 4
SC_WIDTH = D_MODEL
SC_CONV = 3
D_FF = 2816
FFN_CONV = 3
EPS = 1e-6
NEG = -1e30

AB_SPLITS = (RNN_WIDTH, RNN_WIDTH, NSA_HEADS * HEAD_DIM) + (NSA_KV_HEADS * HEAD_DIM,) * 6 + (3 * NSA_HEADS,)
AB_IN = sum(AB_SPLITS)
AB_MIX = RNN_WIDTH + NSA_HEADS * HEAD_DIM

kernel_name = 'hybrid_rglru_nsa_shortconv_trunk'


def rms_norm(x, g):
    xf = x.astype(jnp.float32)
    y = xf * lax.rsqrt(jnp.mean(xf * xf, axis=-1, keepdims=True) + EPS)
    return (y * g.astype(jnp.float32)).astype(x.dtype)


def causal_dwconv(x, w, b):
    K, C = w.shape
    y = lax.conv_general_dilated(x, w[:, None, :].astype(x.dtype), window_strides=(1,),
                                 padding=[(K - 1, 0)], dimension_numbers=('NWC', 'WIO', 'NWC'),
                                 feature_group_count=C)
    return y + b.astype(x.dtype)


def rope_partial(x, pos):
    half = ROPE_DIM // 2
    inv = jnp.power(jnp.float32(ROPE_THETA), -jnp.arange(half, dtype=jnp.float32) / half)
    ang = pos.astype(jnp.float32)[:, None] * inv[None, :]
    cos = jnp.cos(ang)[None, :, None, :]
    sin = jnp.sin(ang)[None, :, None, :]
    x1 = x[..., :half].astype(jnp.float32)
    x2 = x[..., half:ROPE_DIM].astype(jnp.float32)
    rot = jnp.concatenate([x1 * cos - x2 * sin, x2 * cos + x1 * sin], axis=-1).astype(x.dtype)
    return jnp.concatenate([rot, x[..., ROPE_DIM:]], axis=-1)


def rg_lru(x, w_r, b_r, w_i, b_i, lam):
    Bsz, T, W = x.shape
    xb = x.reshape(Bsz, T, RNN_BLOCKS, W // RNN_BLOCKS)
    r = jax.nn.sigmoid(jnp.einsum('btnc,ncd->btnd', xb, w_r).reshape(Bsz, T, W) + b_r)
    i = jax.nn.sigmoid(jnp.einsum('btnc,ncd->btnd', xb, w_i).reshape(Bsz, T, W) + b_i)
    log_a = -LRU_C * r.astype(jnp.float32) * jax.nn.softplus(-lam.astype(jnp.float32))
    a = jnp.exp(log_a)
    u = jnp.sqrt(-jnp.expm1(2.0 * log_a)) * (i * x).astype(jnp.float32)

    def combine(left, right):
        a1, b1 = left
        a2, b2 = right
        return a1 * a2, a2 * b1 + b2

    _, h = lax.associative_scan(combine, (a, u), axis=1)
    return h.astype(x.dtype)


def compress(t, pe, w1, w2):
    Bsz, T, G, hd = t.shape
    n_cmp = (T - CMP_BLOCK) // CMP_STRIDE + 1
    idx = np.arange(n_cmp)[:, None] * CMP_STRIDE + np.arange(CMP_BLOCK)[None, :]
    blk = t[:, idx] + pe[:, None, :].astype(t.dtype)
    blk = blk.transpose(0, 1, 3, 2, 4).reshape(Bsz, n_cmp, G, CMP_BLOCK * hd)
    return jax.nn.gelu(blk @ w1) @ w2, idx[:, -1]


def nsa(q, kc, vc, ks, vs, kw, vw, gate_logits, pe_k, pe_v, wk1, wk2, wv1, wv2):
    Bsz, T = q.shape[:2]
    G, R, hd = NSA_KV_HEADS, GQA_REP, HEAD_DIM
    scale = HEAD_DIM ** -0.5
    pos = jnp.arange(T)
    q_rot = rope_partial(q, pos)
    ks = rope_partial(ks, pos)
    kw = rope_partial(kw, pos)
    k_cmp, cmp_end = compress(kc, pe_k, wk1, wk2)
    v_cmp, _ = compress(vc, pe_v, wv1, wv2)
    n_cmp = k_cmp.shape[1]
    cmp_end = jnp.asarray(cmp_end)
    n_slc = T // SLC_BLOCK
    k_sel = min(SLC_TOPK, n_slc)
    cs = np.arange(n_cmp) * CMP_STRIDE
    js = np.arange(n_slc) * SLC_BLOCK
    overlap = jnp.asarray(((cs[:, None] < js[None, :] + SLC_BLOCK) &
                           (cs[:, None] + CMP_BLOCK > js[None, :])).astype(np.float32))
    ksb = ks.reshape(Bsz, n_slc, SLC_BLOCK, G, hd).transpose(0, 3, 1, 2, 4)
    vsb = vs.reshape(Bsz, n_slc, SLC_BLOCK, G, hd).transpose(0, 3, 1, 2, 4)
    kwp = jnp.pad(kw, ((0, 0), (WINDOW, 0), (0, 0), (0, 0)))
    vwp = jnp.pad(vw, ((0, 0), (WINDOW, 0), (0, 0), (0, 0)))
    bi = jnp.arange(Bsz)[:, None, None, None]
    gi = jnp.arange(G)[None, :, None, None]
    blk_ids = jnp.arange(n_slc)
    f32 = jnp.float32

    def chunk(args):
        qn, qr, gt, s = args
        t = s + jnp.arange(Q_CHUNK)
        qn5 = qn.reshape(Bsz, Q_CHUNK, G, R, hd)
        qr5 = qr.reshape(Bsz, Q_CHUNK, G, R, hd)
        sc = jnp.einsum('bqgrd,bngd->bgrqn', qn5, k_cmp, preferred_element_type=f32) * scale
        mask_c = cmp_end[None, :] <= t[:, None]
        p_c = jnp.where(mask_c, jax.nn.softmax(jnp.where(mask_c, sc, NEG), axis=-1), 0.0)
        o_c = jnp.einsum('bgrqn,bngd->bqgrd', p_c.astype(v_cmp.dtype), v_cmp, preferred_element_type=f32)
        imp = jnp.einsum('bgrqn,nj->bgqj', p_c, overlap)
        forced = (blk_ids[None, :] == 0) | (blk_ids[None, :] == (t // SLC_BLOCK)[:, None])
        imp = jnp.where(forced, imp + FORCE_BONUS, imp)
        imp = jnp.where(blk_ids[None, :] * SLC_BLOCK <= t[:, None], imp, NEG)
        _, sel = lax.top_k(imp, k_sel)
        kg = ksb[bi, gi, sel]
        vg = vsb[bi, gi, sel]
        ss = jnp.einsum('bqgrd,bgqkld->bgrqkl', qr5, kg, preferred_element_type=f32) * scale
        tok = sel[..., None] * SLC_BLOCK + jnp.arange(SLC_BLOCK)
        mask_s = (tok <= t[None, None, :, None, None])[:, :, None]
        ss = jnp.where(mask_s, ss, NEG)
        p_s = jax.nn.softmax(ss.reshape(ss.shape[:4] + (k_sel * SLC_BLOCK,)), axis=-1).reshape(ss.shape)
        o_s = jnp.einsum('bgrqkl,bgqkld->bqgrd', p_s.astype(vg.dtype), vg, preferred_element_type=f32)
        kwin = lax.dynamic_slice_in_dim(kwp, s, WINDOW + Q_CHUNK, axis=1)
        vwin = lax.dynamic_slice_in_dim(vwp, s, WINDOW + Q_CHUNK, axis=1)
        j = s - WINDOW + jnp.arange(WINDOW + Q_CHUNK)
        mask_w = (j[None, :] <= t[:, None]) & (j[None, :] > t[:, None] - WINDOW) & (j[None, :] >= 0)
        sw = jnp.einsum('bqgrd,bjgd->bgrqj', qr5, kwin, preferred_element_type=f32) * scale
        p_w = jax.nn.softmax(jnp.where(mask_w, sw, NEG), axis=-1)
        o_w = jnp.einsum('bgrqj,bjgd->bqgrd', p_w.astype(vwin.dtype), vwin, preferred_element_type=f32)
        g = jax.nn.sigmoid(gt.astype(f32)).reshape(Bsz, Q_CHUNK, G, R, 3)
        o = g[..., 0:1] * o_c + g[..., 1:2] * o_s + g[..., 2:3] * o_w
        return o.reshape(Bsz, Q_CHUNK, NSA_HEADS * hd).astype(q.dtype)

    n_chunk = T // Q_CHUNK
    to_chunks = lambda a: a.reshape((Bsz, n_chunk, Q_CHUNK) + a.shape[2:]).swapaxes(0, 1)
    starts = jnp.arange(n_chunk) * Q_CHUNK
    out = lax.map(chunk, (to_chunks(q), to_chunks(q_rot), to_chunks(gate_logits), starts))
    return out.swapaxes(0, 1).reshape(Bsz, T, NSA_HEADS * hd)


def mixer_ab(h, w_in, conv_w, conv_b, w_r, b_r, w_i, b_i, lam, pe_k, pe_v, wk1, wk2, wv1, wv2, w_out):
    Bsz, T, _ = h.shape
    splits = np.cumsum(AB_SPLITS)[:-1].tolist()
    xr, gr, q, kc, vc, ks, vs, kw, vw, gl = jnp.split(h @ w_in, splits, axis=-1)
    y_rnn = rg_lru(causal_dwconv(xr, conv_w, conv_b), w_r, b_r, w_i, b_i, lam) * jax.nn.gelu(gr)
    kv = lambda a: a.reshape(Bsz, T, NSA_KV_HEADS, HEAD_DIM)
    y_att = nsa(q.reshape(Bsz, T, NSA_HEADS, HEAD_DIM), kv(kc), kv(vc), kv(ks), kv(vs), kv(kw), kv(vw),
                gl.reshape(Bsz, T, NSA_HEADS, 3), pe_k, pe_v, wk1, wk2, wv1, wv2)
    return jnp.concatenate([y_rnn, y_att], axis=-1) @ w_out


def mixer_c(h, w_in, conv_w, conv_b, w_out):
    bg, cg, v = jnp.split(h @ w_in, 3, axis=-1)
    return (bg * causal_dwconv(cg * v, conv_w, conv_b)) @ w_out


def conv_ffn(h, w_gate, w_up, conv_w, conv_b, w_down):
    g = causal_dwconv(h @ w_gate, conv_w, conv_b)
    return (jax.nn.silu(g) * (h @ w_up)) @ w_down


def setup_inputs(seed: int = 0) -> dict:
    key = jax.random.key(seed)
    keys = iter(jax.random.split(key, 48))
    NE = (DEPTH + 1) // 2
    NO = DEPTH // 2
    f32 = jnp.float32

    def w(shape, fan_in, scale=1.0):
        return jax.random.normal(next(keys), shape, f32) * (scale * fan_in ** -0.5)

    def small(shape):
        return 0.02 * jax.random.normal(next(keys), shape, f32)

    def gain(shape):
        return 1.0 + small(shape)

    x = jax.random.normal(next(keys), (BATCH, SEQ, D_MODEL), f32)
    c = jax.random.normal(next(keys), (BATCH, D_MODEL), f32)
    a_target = jax.random.uniform(next(keys), (NE, RNN_WIDTH), f32, 0.9, 0.999)
    s = a_target ** (1.0 / LRU_C)
    lam = jnp.log(s) - jnp.log1p(-s)
    bw = RNN_WIDTH // RNN_BLOCKS
    return {
        'x': x,
        'c': c,
        'mod_w': w((DEPTH, D_MODEL, 6 * D_MODEL), D_MODEL, 0.5),
        'mod_b': small((DEPTH, 6 * D_MODEL)),
        'norm_mix_pre': gain((DEPTH, D_MODEL)),
        'norm_mix_post': gain((DEPTH, D_MODEL)),
        'norm_ffn_pre': gain((DEPTH, D_MODEL)),
        'norm_ffn_post': gain((DEPTH, D_MODEL)),
        'ab_w_in': w((NE, D_MODEL, AB_IN), D_MODEL),
        'ab_conv_w': w((NE, RNN_CONV, RNN_WIDTH), RNN_CONV),
        'ab_conv_b': small((NE, RNN_WIDTH)),
        'lru_w_r': w((NE, RNN_BLOCKS, bw, bw), bw),
        'lru_b_r': small((NE, RNN_WIDTH)),
        'lru_w_i': w((NE, RNN_BLOCKS, bw, bw), bw),
        'lru_b_i': small((NE, RNN_WIDTH)),
        'lru_lam': lam,
        'cmp_pe_k': small((NE, CMP_BLOCK, HEAD_DIM)),
        'cmp_pe_v': small((NE, CMP_BLOCK, HEAD_DIM)),
        'cmp_wk1': w((NE, CMP_BLOCK * HEAD_DIM, CMP_HIDDEN), CMP_BLOCK * HEAD_DIM),
        'cmp_wk2': w((NE, CMP_HIDDEN, HEAD_DIM), CMP_HIDDEN),
        'cmp_wv1': w((NE, CMP_BLOCK * HEAD_DIM, CMP_HIDDEN), CMP_BLOCK * HEAD_DIM),
        'cmp_wv2': w((NE, CMP_HIDDEN, HEAD_DIM), CMP_HIDDEN),
        'ab_w_out': w((NE, AB_MIX, D_MODEL), AB_MIX),
        'sc_w_in': w((NO, D_MODEL, 3 * SC_WIDTH), D_MODEL),
        'sc_conv_w': w((NO, SC_CONV, SC_WIDTH), SC_CONV),
        'sc_conv_b': small((NO, SC_WIDTH)),
        'sc_w_out': w((NO, SC_WIDTH, D_MODEL), SC_WIDTH),
        'ffn_w_gate': w((DEPTH, D_MODEL, D_FF), D_MODEL),
        'ffn_w_up': w((DEPTH, D_MODEL, D_FF), D_MODEL),
        'ffn_conv_w': w((DEPTH, FFN_CONV, D_FF), FFN_CONV),
        'ffn_conv_b': small((DEPTH, D_FF)),
        'ffn_w_down': w((DEPTH, D_FF, D_MODEL), D_FF),
    }


def reference(x, c, mod_w, mod_b, norm_mix_pre, norm_mix_post, norm_ffn_pre, norm_ffn_post,
              ab_w_in, ab_conv_w, ab_conv_b, lru_w_r, lru_b_r, lru_w_i, lru_b_i, lru_lam,
              cmp_pe_k, cmp_pe_v, cmp_wk1, cmp_wk2, cmp_wv1, cmp_wv2, ab_w_out,
              sc_w_in, sc_conv_w, sc_conv_b, sc_w_out,
              ffn_w_gate, ffn_w_up, ffn_conv_w, ffn_conv_b, ffn_w_down):
    c_act = jax.nn.silu(c)
    for i in range(DEPTH):
        mod = c_act @ mod_w[i] + mod_b[i]
        sh_m, sc_m, g_m, sh_f, sc_f, g_f = jnp.split(mod[:, None, :], 6, axis=-1)
        h = rms_norm(x, norm_mix_pre[i]) * (1.0 + sc_m) + sh_m
        if i % 2 == 0:
            e = i // 2
            y = mixer_ab(h, ab_w_in[e], ab_conv_w[e], ab_conv_b[e], lru_w_r[e], lru_b_r[e],
                         lru_w_i[e], lru_b_i[e], lru_lam[e], cmp_pe_k[e], cmp_pe_v[e],
                         cmp_wk1[e], cmp_wk2[e], cmp_wv1[e], cmp_wv2[e], ab_w_out[e])
        else:
            o = i // 2
            y = mixer_c(h, sc_w_in[o], sc_conv_w[o], sc_conv_b[o], sc_w_out[o])
        x = x + (1.0 + g_m) * rms_norm(y, norm_mix_post[i])
        h = rms_norm(x, norm_ffn_pre[i]) * (1.0 + sc_f) + sh_f
        y = conv_ffn(h, ffn_w_gate[i], ffn_w_up[i], ffn_conv_w[i], ffn_conv_b[i], ffn_w_down[i])
        x = x + (1.0 + g_f) * rms_norm(y, norm_ffn_post[i])
    return x
```

```python
import numpy as np
from contextlib import ExitStack
import concourse.bass as bass
import concourse.mybir as mybir
from concourse.bass_utils import run_bass_kernel_spmd

F32 = mybir.dt.float32
BF16 = mybir.dt.bfloat16
AF = mybir.ActivationFunctionType
ALU = mybir.AluOpType
AX = mybir.AxisListType

D = 1024
T = 2048
TT = 512
NTILE = T // TT
DFF = 2816
NFC = DFF // 128
EPS = 1e-6
MASKV = -4000.0
NDSEM = 24
NSLOT = 4
SLOTW = 3072
NPV = 864
PV_GAIN = 0
PV_MODB = 128
PV_EVEN = 320
PV_ODD = 448
PV_FFN = 512


class Buf:
    __slots__ = ("name", "w", "r", "excl")

    def __init__(s, name, excl=False):
        s.name = name
        s.w = None
        s.r = {}
        s.excl = excl


class Sched:
    def __init__(s, nc, es):
        s.nc = nc
        s.eng = {'pe': nc.tensor, 'act': nc.scalar, 'dve': nc.vector, 'pool': nc.gpsimd, 'sp': nc.sync}
        s.sem = {k: es.enter_context(nc.semaphore("s_" + k)) for k in ['pe', 'act', 'dve', 'pool']}
        s.cnt = {k: 0 for k in s.sem}
        s.dsem = [es.enter_context(nc.semaphore(f"d{i}")) for i in range(NDSEM)]
        s.dcnt = [0] * NDSEM
        s.dnext = {'pool': 0, 'sp': 0}
        s.waited = {e: {} for e in s.eng}
        s.nins = 0
        s.nwait = 0

    def _wait(s, e, deps):
        need = {}
        for k, v in deps:
            if v > need.get(k, 0):
                need[k] = v
        for k, v in need.items():
            if k == e and e == 'pe':
                continue
            if s.waited[e].get(k, 0) >= v:
                continue
            semh = s.sem[k] if isinstance(k, str) else s.dsem[k[1]]
            s.eng[e].wait_ge(semh, v)
            s.nwait += 1
            s.waited[e][k] = v

    def deps_for(s, reads, writes):
        d = []
        for b in reads:
            if b.w:
                d.append(b.w)
        for b in writes:
            if b.w:
                d.append(b.w)
            d.extend(b.r.items())
        return d

    def _mark(s, tag, reads, writes):
        k, v = tag
        for b in reads:
            if b.r.get(k, 0) < v:
                b.r[k] = v
        for b in writes:
            b.w = tag
            b.r = {}

    def op(s, e, fn, reads=(), writes=()):
        if any(b.excl for b in reads):
            writes = list(writes) + [b for b in reads if b.excl]
            reads = [b for b in reads if not b.excl]
        s._wait(e, s.deps_for(reads, writes))
        ins = fn()
        s.nins += 1
        s.cnt[e] += 1
        ins.then_inc(s.sem[e], 1)
        s._mark((e, s.cnt[e]), reads, writes)
        return ins

    def dma(s, e, out, in_, reads=(), writes=()):
        h = NDSEM // 2
        j = s.dnext[e]
        s.dnext[e] = (j + 1) % h
        i = j if e == 'pool' else h + j
        deps = s.deps_for(reads, writes)
        if s.dcnt[i] > 0:
            deps.append((('d', i), s.dcnt[i]))
        s._wait(e, deps)
        ins = s.eng[e].dma_start(out=out, in_=in_)
        s.nins += 1
        s.dcnt[i] += 16
        ins.then_inc(s.dsem[i], 16)
        s._mark((('d', i), s.dcnt[i]), reads, writes)
        return ins

    def finish(s, e, bufs):
        s._wait(e, [b.w for b in bufs if b.w])


def _tile_k(w, kc):
    K, N = w.shape
    return w.reshape(kc, 128, N).transpose(1, 0, 2)


def host_weights(inp):
    f = np.float32
    NE, NO = 2, 2
    out = {}
    w_in = inp['ab_w_in']
    w_rnn = np.zeros((NE, 8, 128, 2, 8, 128), f)
    w_q = np.zeros((NE, 4, 128, 8, 256), f)
    w_kv = np.zeros((NE, 4, 128, 8, 4, 64), f)
    w_vs = np.zeros((NE, 128, 8, 256), f)
    w_vw = np.zeros((NE, 128, 8, 256), f)
    w_gl = np.zeros((NE, 128, 8, 48), f)
    w_bd = np.zeros((NE, 128, 8, 2, 128), f)
    w_c1 = np.zeros((NE, 2, 2, 64, 16, 128), f)
    w_cm = np.zeros((NE, 128, 640), f)
    w_out = np.zeros((NE, 8, 128, 16, 128), f)
    for e in range(NE):
        wt = _tile_k(w_in[e], 8)
        for c in range(8):
            w_rnn[e, c, :, 0] = wt[:, :, c * 128:(c + 1) * 128]
            w_rnn[e, c, :, 1] = wt[:, :, 1024 + c * 128:1024 + (c + 1) * 128]
        for g in range(4):
            w_q[e, g] = wt[:, :, 2048 + g * 256:2048 + (g + 1) * 256]
            for ti, off in enumerate((3584, 4096, 3072, 3328)):
                w_kv[e, g, :, :, ti, :] = wt[:, :, off + g * 64:off + (g + 1) * 64]
        w_vs[e] = wt[:, :, 3840:4096]
        w_vw[e] = wt[:, :, 4352:4608]
        w_gl[e] = wt[:, :, 4608:4656]
        for c in range(8):
            for hb in range(2):
                n = 2 * c + hb
                w_bd[e, hb * 64:(hb + 1) * 64, c, 0, hb * 64:(hb + 1) * 64] = inp['lru_w_r'][e, n]
                w_bd[e, hb * 64:(hb + 1) * 64, c, 1, hb * 64:(hb + 1) * 64] = inp['lru_w_i'][e, n]
        for kv, nm in enumerate(('cmp_wk1', 'cmp_wv1')):
            w1 = inp[nm][e].reshape(32, 64, 128)
            for hf in range(2):
                w_c1[e, kv, hf] = w1[hf * 16:(hf + 1) * 16].transpose(1, 0, 2)
        w_cm[e, :, 0:64] = inp['cmp_wk2'][e]
        w_cm[e, :, 64:128] = inp['cmp_wv2'][e]
        for kv, nm in enumerate(('cmp_pe_k', 'cmp_pe_v')):
            pe = inp[nm][e]
            w_cm[e, 0:64, 128 + kv * 256:128 + (kv + 1) * 256] = np.repeat(pe.T[:, :, None], 8, axis=2).reshape(64, 256)
        wo = _tile_k(inp['ab_w_out'][e], 16)
        for oc in range(8):
            w_out[e, oc] = wo[:, :, oc * 128:(oc + 1) * 128]
    out['w_rnn'] = w_rnn.reshape(NE, 8, 128, 2048)
    out['w_q'] = w_q.reshape(NE, 4, 128, 2048)
    out['w_kv'] = w_kv.reshape(NE, 4, 128, 2048)
    out['w_vs'] = w_vs.reshape(NE, 128, 2048)
    out['w_vw'] = w_vw.reshape(NE, 128, 2048)
    out['w_gl'] = w_gl.reshape(NE, 128, 384)
    out['w_bd'] = w_bd.reshape(NE, 128, 2048)
    out['w_c1'] = w_c1.reshape(NE, 2, 2, 64, 2048)
    out['w_cm'] = w_cm
    out['w_out'] = w_out.reshape(NE, 8, 128, 2048)
    w_sc = np.zeros((NO, 8, 128, 3, 8, 128), f)
    w_sco = np.zeros((NO, 8, 128, 8, 128), f)
    for o in range(NO):
        wt = _tile_k(inp['sc_w_in'][o], 8)
        for c in range(8):
            for k3 in range(3):
                w_sc[o, c, :, k3] = wt[:, :, k3 * 1024 + c * 128:k3 * 1024 + (c + 1) * 128]
        wo = _tile_k(inp['sc_w_out'][o], 8)
        for oc in range(8):
            w_sco[o, oc] = wo[:, :, oc * 128:(oc + 1) * 128]
    out['w_sc'] = w_sc.reshape(NO, 8, 128, 3072)
    out['w_sco'] = w_sco.reshape(NO, 8, 128, 1024)
    w_gu = np.zeros((4, NFC, 128, 2, 8, 128), f)
    w_dn = np.zeros((4, 8, 128, NFC, 128), f)
    modw = np.zeros((4, 24, 128, 2, 8, 128), f)
    for L in range(4):
        wg = _tile_k(inp['ffn_w_gate'][L], 8)
        wu = _tile_k(inp['ffn_w_up'][L], 8)
        for c in range(NFC):
            w_gu[L, c, :, 0] = wg[:, :, c * 128:(c + 1) * 128]
            w_gu[L, c, :, 1] = wu[:, :, c * 128:(c + 1) * 128]
        wd = _tile_k(inp['ffn_w_down'][L], NFC)
        for oc in range(8):
            w_dn[L, oc] = wd[:, :, oc * 128:(oc + 1) * 128]
        wm = _tile_k(inp['mod_w'][L], 8)
        for jp in range(24):
            for jj in range(2):
                j = 2 * jp + jj
                modw[L, jp, :, jj] = wm[:, :, j * 128:(j + 1) * 128]
    out['w_gu'] = w_gu.reshape(4, NFC, 128, 2048)
    out['w_dn'] = w_dn.reshape(4, 8, 128, NFC * 128)
    out['modw'] = modw.reshape(4, 24, 128, 2048)
    pv = np.zeros((128, NPV), f)

    def fm(v):
        return v.reshape(8, 128).T

    for kind, nm in enumerate(('norm_mix_pre', 'norm_mix_post', 'norm_ffn_pre', 'norm_ffn_post')):
        for L in range(4):
            pv[:, PV_GAIN + kind * 32 + L * 8:PV_GAIN + kind * 32 + L * 8 + 8] = fm(inp[nm][L])
    for L in range(4):
        pv[:, PV_MODB + L * 48:PV_MODB + (L + 1) * 48] = inp['mod_b'][L].reshape(48, 128).T
    for e in range(NE):
        b = PV_EVEN + e * 64
        cw = inp['ab_conv_w'][e]
        pv[:, b:b + 32] = cw.reshape(4, 8, 128).transpose(2, 1, 0).reshape(128, 32)
        pv[:, b + 32:b + 40] = fm(inp['ab_conv_b'][e])
        pv[:, b + 40:b + 48] = fm(inp['lru_b_r'][e])
        pv[:, b + 48:b + 56] = fm(inp['lru_b_i'][e])
        pv[:, b + 56:b + 64] = fm(inp['lru_lam'][e])
    for o in range(NO):
        b = PV_ODD + o * 32
        cw = inp['sc_conv_w'][o]
        pv[:, b:b + 24] = cw.reshape(3, 8, 128).transpose(2, 1, 0).reshape(128, 24)
        pv[:, b + 24:b + 32] = fm(inp['sc_conv_b'][o])
    for L in range(4):
        b = PV_FFN + L * 88
        cw = inp['ffn_conv_w'][L]
        pv[:, b:b + 66] = cw.reshape(3, NFC, 128).transpose(2, 1, 0).reshape(128, 66)
        pv[:, b + 66:b + 88] = inp['ffn_conv_b'][L].reshape(NFC, 128).T
    out['pvec'] = pv
    return out


def host_consts():
    f = np.float32
    c = {}
    c['c_id'] = np.eye(128, dtype=f)
    kk = np.arange(128)[:, None]
    tt = np.arange(128)[None, :]
    c['c_mc'] = np.where(kk <= tt, 0.0, MASKV).astype(f)
    c['c_mu'] = np.where(kk > tt, 0.0, MASKV).astype(f)
    key = np.arange(T)[None, :]
    c['c_E'] = (key // 64 == np.arange(32)[:, None]).astype(f)
    j = np.arange(128)[:, None]
    t = np.arange(T)[None, :]
    c['c_cm'] = np.where((j >= 1) & (16 * j + 15 <= t), 0.0, MASKV).astype(f)
    tabs = (np.arange(16)[None, :, None] * 128 + np.arange(128)[:, None, None])
    jb = np.arange(32)[None, None, :]
    forced = (jb == 0) | (jb == tabs // 64)
    valid = jb * 64 <= tabs
    bon = np.where(valid, np.where(forced, 1.0e4, 0.0), -1.0e30).astype(f)
    c['c_bon'] = bon.reshape(128, 512)
    n = np.arange(128)[:, None] - 1
    jb2 = np.arange(32)[None, :]
    cov = ((n >= 0) & (16 * n < 64 * jb2 + 64) & (16 * n + 32 > 64 * jb2)).astype(f)
    c['c_cov'] = cov
    half = 8
    inv = np.power(np.float32(500000.0), -np.arange(half, dtype=f) / half).astype(f)
    ang = (np.arange(T, dtype=f)[None, :] * inv[:, None]).astype(f)
    cos = np.cos(ang).astype(f)
    sin = np.sin(ang).astype(f)
    rope = np.zeros((64, 2, T), f)
    rope[:, 0, :] = 1.0
    rope[0:8, 0, :] = cos
    rope[8:16, 0, :] = cos
    rope[0:8, 1, :] = -sin
    rope[8:16, 1, :] = sin
    c['c_rope'] = rope
    sw = np.zeros((64, 64), f)
    for m in range(8):
        sw[m + 8, m] = 1.0
        sw[m, m + 8] = 1.0
    c['c_sw'] = sw
    c['c_ones'] = np.ones((128, 128), f)
    return c


WSHAPES = {
    'w_rnn': (2, 8, 128, 2048), 'w_q': (2, 4, 128, 2048), 'w_kv': (2, 4, 128, 2048),
    'w_vs': (2, 128, 2048), 'w_vw': (2, 128, 2048), 'w_gl': (2, 128, 384), 'w_bd': (2, 128, 2048),
    'w_c1': (2, 2, 2, 64, 2048), 'w_cm': (2, 128, 640), 'w_out': (2, 8, 128, 2048),
    'w_sc': (2, 8, 128, 3072), 'w_sco': (2, 8, 128, 1024), 'w_gu': (4, NFC, 128, 2048),
    'w_dn': (4, 8, 128, NFC * 128),
}
CSHAPES = {
    'c_id': (128, 128), 'c_mc': (128, 128), 'c_mu': (128, 128), 'c_E': (32, T), 'c_cm': (128, T),
    'c_bon': (128, 512), 'c_cov': (128, 32), 'c_rope': (64, 2, T), 'c_sw': (64, 64), 'c_ones': (128, 128),
}


class Prog:
    def __init__(s, NSEQ, NLAYER, taps=()):
        s.NSEQ = NSEQ
        s.NLAYER = NLAYER
        s.taps = set(taps)
        s.tapbufs = []
        import os as _os
        s.stage = int(_os.environ.get("KSTAGE", "99"))
        s.ntile = int(_os.environ.get("KTILES", str(NTILE)))
        s.nc = bass.Bass("TRN2", target_bir_lowering=False)
        s.es = ExitStack()

    def sb(s, name, shape, dt):
        return s.es.enter_context(s.nc.sbuf_tensor(name, list(shape), dt))

    def din(s, name, shape, dt=F32):
        return s.nc.dram_tensor(name, list(shape), dt, kind="ExternalInput").ap()

    def tap(s, name, ap, buf, shape):
        if name not in s.taps:
            return
        o = s.nc.dram_tensor("tap_" + name, list(shape), F32, kind="ExternalOutput").ap()
        b = Buf("tap_" + name)
        s.S.dma('pool', o, ap, reads=[buf], writes=[b])
        s.tapbufs.append(b)

    def build(s):
        nc = s.nc
        NSEQ, NLAYER = s.NSEQ, s.NLAYER
        with s.es:
            s.S = S = Sched(nc, s.es)
            s.xT = s.din("xT", [NSEQ, D, T])
            s.cT = s.din("cT", [128, 8 * NSEQ])
            s.pvec_d = s.din("pvec", [128, NPV])
            s.modw_d = s.din("modw", [4, 24, 128, 2048])
            s.wd = {k: s.din(k, sh) for k, sh in WSHAPES.items()}
            s.wb = {k: nc.dram_tensor(k + "_b", list(sh), BF16, kind="Internal").ap() for k, sh in WSHAPES.items()}
            s.wbbuf = {k: Buf(k + "_b") for k in WSHAPES}
            s.cd = {k: s.din(k, sh) for k, sh in CSHAPES.items()}
            s.yT = nc.dram_tensor("yT", [NSEQ, D, T], F32, kind="ExternalOutput").ap()
            s.xs = [nc.dram_tensor(f"xs{i}", [D, T], F32, kind="Internal").ap() for i in range(2)]
            s.xsbuf = [[[Buf(f"xs{i}_{t}_{c}") for c in range(8)] for t in range(NTILE)] for i in range(2)]
            s.ybuf_out = []
            s.ps = [s.es.enter_context(nc.psum_tensor(f"ps{i}", [128, 512], F32)) for i in range(8)]
            s.psb = [Buf(f"ps{i}", excl=True) for i in range(8)]
            s.rot = {}
            s.alloc_sbuf()
            s.prepass()
            for b in range(NSEQ):
                for L in range(NLAYER):
                    s.layer(b, L)
            S.finish('pool', s.ybuf_out + s.tapbufs)
        return nc

    def rotate(s, key, items):
        i = s.rot.get(key, 0)
        s.rot[key] = i + 1
        return items[i % len(items)]

    def bank(s, key, ids):
        i = s.rotate(key, ids)
        return s.ps[i], s.psb[i]

    def alloc_sbuf(s):
        sb = s.sb
        s.xt = sb("xt", [128, 8, TT], F32)
        s.xtb = [Buf(f"xt{c}") for c in range(8)]
        s.hT = sb("hT", [128, 8, TT], BF16)
        s.hTb = [Buf(f"hT{c}") for c in range(8)]
        s.big = sb("big", [128, NFC, TT], BF16)
        s.bigb = [Buf(f"big{c}") for c in range(NFC)]
        s.ybuf = sb("ybuf", [128, 8, TT], F32)
        s.ybufb = [Buf(f"ybuf{c}") for c in range(8)]
        s.tmp = sb("tmp", [128, 7, TT], F32)
        s.tmpb = [Buf(f"tmp{c}") for c in range(7)]
        s.tb16 = sb("tb16", [128, 4, TT], BF16)
        s.tb16b = [Buf(f"tb16_{c}") for c in range(4)]
        s.cin = sb("cin", [128, 2, TT + 4], F32)
        s.cinb = [Buf("cin0"), Buf("cin1")]
        s.pt = sb("pt", [128, 4, TT], BF16)
        s.ptb = [Buf(f"pt{c}") for c in range(4)]
        s.ring = sb("ring", [128, NSLOT, SLOTW], BF16)
        s.ringb = [Buf(f"ring{c}") for c in range(NSLOT)]
        s.ksA = sb("ksA", [128, 4, T], BF16)
        s.ksAb = Buf("ksA")
        s.kwT = sb("kwT", [64, 4, 1024], BF16)
        s.kwTb = Buf("kwT")
        s.vsA = sb("vsA", [128, 16, 4, 66], BF16)
        s.vsAb = Buf("vsA")
        s.vwA = sb("vwA", [128, 8, 4, 66], BF16)
        s.vwAb = Buf("vwA")
        s.hkT = sb("hkT", [128, 4, 128], BF16)
        s.hkTb = Buf("hkT")
        s.hvT = sb("hvT", [128, 4, 128], BF16)
        s.hvTb = Buf("hvT")
        s.kcmpT = sb("kcmpT", [64, 4, 128], BF16)
        s.kcmpTb = Buf("kcmpT")
        s.vcmpA = sb("vcmpA", [128, 4, 98], BF16)
        s.vcmpAb = Buf("vcmpA")
        s.kcT = sb("kcT", [64, 4, 528], BF16)
        s.kcTb = Buf("kcT")
        s.vcT = sb("vcT", [64, 4, 528], BF16)
        s.vcTb = Buf("vcT")
        s.qA = sb("qA", [64, 4, TT], BF16)
        s.qAb = Buf("qA")
        s.qrA = sb("qrA", [96, 4, TT], BF16)
        s.qrAb = Buf("qrA")
        s.yatt = sb("yatt", [128, 4, 1024], BF16)
        s.yattb = [Buf(f"yatt{c}") for c in range(4)]
        s.gsig = sb("gsig", [128, 4, 48], F32)
        s.gsigb = Buf("gsig")
        s.rope = sb("rope", [64, 2, TT], F32)
        s.ropeb = Buf("rope")
        s.cmask = sb("cmask", [128, TT], BF16)
        s.cmaskb = Buf("cmask")
        s.bon = sb("bon", [128, 4, 32], F32)
        s.bonb = Buf("bon")
        s.ident = sb("ident", [128, 128], BF16)
        s.ones = sb("ones", [128, 128], BF16)
        s.mc = sb("mc", [128, 128], BF16)
        s.mu = sb("mu", [128, 128], BF16)
        s.swm = sb("swm", [64, 64], BF16)
        s.constb = Buf("consts")
        s.Mq = sb("Mq", [128, 96], BF16)
        s.Mqb = Buf("Mq")
        s.pvec = sb("pvec_s", [128, NPV], F32)
        s.modT = sb("modT", [128, 4, 48, s.NSEQ], F32)
        s.coef = sb("coef", [128, 4, 4, 8, s.NSEQ], F32)
        s.cl = sb("cl", [128, 2, 8], F32)
        s.parb = Buf("params")
        s.cact = sb("cact", [128, 8, 8], BF16)
        s.ccar = sb("ccar", [128, 8, 4], F32)
        s.ccarb = Buf("ccar")
        s.hst = sb("hst", [128, 8], F32)
        s.hstb = Buf("hst")
        s.fcar = sb("fcar", [128, NFC, 2], F32)
        s.fcarb = Buf("fcar")
        s.cbias = sb("cbias", [128, 2], F32)
        s.cbiasb = Buf("cbias")
        s.sm = sb("sm", [128, 8, 4], F32)
        s.smb = [Buf(f"sm{c}") for c in range(8)]
        s.imp = sb("imp", [128, 2, 40], F32)
        s.impb = [Buf("imp0"), Buf("imp1")]
        s.oc = sb("oc", [128, 5, 256], F32)
        s.ocb = [Buf(f"oc{c}") for c in range(5)]
        s.bdw = sb("bdw", [128, 2048], BF16)
        s.bdwb = Buf("bdw")
        s.wcm = sb("wcm", [128, 640], BF16)
        s.wcmb = Buf("wcm")
        s.rstd = sb("rstd", [128, TT], F32)
        s.rstdb = Buf("rstd")
        s.Mq2 = sb("Mq2", [128, 96], BF16)
        s.Mq2b = Buf("Mq2")

    def T32(s):
        i = s.rotate('tmp', list(range(7)))
        return s.tmp[:, i, :], s.tmpb[i]

    def T16(s):
        i = s.rotate('tb16', list(range(4)))
        return s.tb16[:, i, :], s.tb16b[i]

    def PT(s):
        i = s.rotate('pt', list(range(4)))
        return s.pt[:, i, :], s.ptb[i]

    def SM(s):
        i = s.rotate('sm', list(range(8)))
        return s.sm[:, i, :], s.smb[i]

    def wload(s, src, P, n, srcbuf=None, eng='sp'):
        i = s.rotate('ring', list(range(NSLOT)))
        dst = s.ring[0:P, i, 0:n]
        s.S.dma(eng, dst, src, reads=[srcbuf] if srcbuf is not None else [], writes=[s.ringb[i]])
        return s.ring[:, i, :], s.ringb[i]

    def pv(s, off, n=1):
        return s.pvec[:, off:off + n]

    def prepass(s):
        nc, S = s.nc, s.S
        NSEQ = s.NSEQ
        for k, sh in WSHAPES.items():
            src, dst = s.wd[k], s.wb[k]
            n0 = sh[0]
            for i0 in range(n0):
                if len(sh) >= 4:
                    for i1 in range(0, sh[1], 8):
                        i2 = min(i1 + 8, sh[1])
                        S.dma('pool', dst[i0, i1:i2], src[i0, i1:i2], writes=[])
                else:
                    S.dma('pool', dst[i0], src[i0], writes=[])
        allw = [(('d', i), S.dcnt[i]) for i in range(NDSEM) if S.dcnt[i] > 0]
        S._wait('sp', allw)
        S._wait('pool', allw)
        cb = s.constb
        S.dma('pool', s.ident[:], s.cd['c_id'], writes=[cb])
        S.dma('pool', s.ones[:], s.cd['c_ones'], writes=[])
        S.dma('pool', s.mc[:], s.cd['c_mc'], writes=[])
        S.dma('pool', s.mu[:], s.cd['c_mu'], writes=[])
        S.dma('pool', s.swm[:], s.cd['c_sw'], writes=[])
        S.dma('pool', s.pvec[:], s.pvec_d, writes=[])
        ct = s.sb("ct", [128, 8 * NSEQ], F32)
        S.dma('pool', ct[:], s.cT, writes=[])
        for g in range(4):
            S.dma('pool', s.ksA[64:96, g, :], s.cd['c_E'], writes=[])
            S.dma('pool', s.vcmpA[:, g, 65:97], s.cd['c_cov'], writes=[])
        allw = [(('d', i), S.dcnt[i]) for i in range(NDSEM) if S.dcnt[i] > 0]
        for e in ('pe', 'act', 'dve', 'pool'):
            S._wait(e, allw)
        S.op('dve', lambda: nc.vector.memset(s.vsA[:, :, :, 64:65], 1.0), writes=[s.vsAb])
        S.op('dve', lambda: nc.vector.memset(s.vwA[:, :, :, 64:65], 1.0), writes=[s.vwAb])
        S.op('dve', lambda: nc.vector.memset(s.vcmpA[:, :, 64:65], 1.0), writes=[s.vcmpAb])
        S.op('dve', lambda: nc.vector.memset(s.Mq[:], 0.0), writes=[s.Mqb])
        S.op('dve', lambda: nc.vector.memset(s.Mq2[:], 0.0), writes=[s.Mq2b])
        S.op('dve', lambda: nc.vector.memset(s.kwT[:], 0.0), writes=[s.kwTb])
        S.op('dve', lambda: nc.vector.memset(s.vwA[:, :, :, 0:64], 0.0), writes=[s.vwAb])
        pb = s.parb
        S.op('dve', lambda: nc.vector.memset(s.cact[:], 0.0), writes=[pb])
        S.op('act', lambda: nc.scalar.activation(out=s.cact[:, :, 0:NSEQ], in_=ct[:].rearrange("p (a b) -> p a b", a=8), func=AF.Silu), reads=[pb], writes=[pb])
        for e in range(2):
            lam = s.pv(PV_EVEN + e * 64 + 56, 8)
            S.op('act', lambda: nc.scalar.activation(out=s.cl[:, e, :], in_=lam, func=AF.Exp, scale=-1.0), writes=[pb])
            S.op('act', lambda: nc.scalar.activation(out=s.cl[:, e, :], in_=s.cl[:, e, :], func=AF.Ln, bias=1.0), writes=[pb])
            S.op('act', lambda: nc.scalar.mul(s.cl[:, e, :], s.cl[:, e, :], -8.0), writes=[pb])
        for L in range(s.NLAYER):
            for jp in range(24):
                W, Wb = s.wload(s.modw_d[L, jp], 128, 2048, eng='pool')
                Wv = W[:, 0:2048].rearrange("p (j k m) -> p j k m", j=2, k=8)
                for jj in range(2):
                    j = 2 * jp + jj
                    pm, pmb = s.bank('pp', [0, 1, 2, 3])

                    def f():
                        for kc in range(8):
                            ins = nc.tensor.matmul(pm[:, 0:8], Wv[:, jj, kc, :], s.cact[:, kc, :], start=(kc == 0), stop=(kc == 7))
                        return ins
                    S.op('pe', f, reads=[Wb, pb], writes=[pmb])
                    S.op('act', lambda: nc.scalar.activation(out=s.modT[:, L, j, :], in_=pm[:, 0:NSEQ], func=AF.Identity,
                                                              bias=s.pv(PV_MODB + L * 48 + j), scale=1.0), reads=[pmb], writes=[pb])
            for kind, (gk, j0) in enumerate(((0, 8), (1, 16), (2, 32), (3, 40))):
                gain = s.pvec[:, PV_GAIN + gk * 32 + L * 8:PV_GAIN + gk * 32 + L * 8 + 8]
                S.op('dve', lambda: nc.vector.scalar_tensor_tensor(
                    s.coef[:, L, kind, :, :], s.modT[:, L, j0:j0 + 8, :], 1.0,
                    gain.unsqueeze(2).to_broadcast([128, 8, NSEQ]), ALU.add, ALU.mult), reads=[pb], writes=[pb])

    def rstd_from_ss(s, pss, pssb):
        nc, S = s.nc, s.S
        r, rb = s.rstd[:], s.rstdb
        S.op('act', lambda: nc.scalar.activation(out=r, in_=pss[:], func=AF.Sqrt, scale=1.0 / D, bias=EPS), reads=[pssb], writes=[rb])
        S.op('dve', lambda: nc.vector.reciprocal(r, r), reads=[rb], writes=[rb])
        return r, rb

    def prenorm(s, A, B):
        nc, S = s.nc, s.S
        pss, pssb = s.ps[6], s.psb[6]
        for c in range(8):
            q, qb = s.T16()
            S.op('act', lambda: nc.scalar.activation(out=q, in_=s.xt[:, c, :], func=AF.Square), reads=[s.xtb[c]], writes=[qb])
            S.op('pe', lambda: nc.tensor.matmul(pss[:], s.ones[:], q, start=(c == 0), stop=(c == 7)), reads=[qb, s.constb], writes=[pssb])
        r, rb = s.rstd_from_ss(pss, pssb)
        for c in range(8):
            t, tb = s.T32()
            S.op('dve', lambda: nc.vector.tensor_tensor(t, s.xt[:, c, :], r, ALU.mult), reads=[s.xtb[c], rb], writes=[tb])
            S.op('act', lambda: nc.scalar.activation(out=s.hT[:, c, :], in_=t, func=AF.Identity, scale=A(c), bias=B(c)),
                 reads=[tb, s.parb], writes=[s.hTb[c]])

    def out_proj_residual(s, nk, wfam, widx, GP):
        nc, S = s.nc, s.S
        pss, pssb = s.ps[6], s.psb[6]
        for oc in range(8):
            W, Wb = s.wload(s.wb[wfam][widx][oc], 128, nk * 128, srcbuf=None)
            Wv = W[:, 0:nk * 128].rearrange("p (k m) -> p k m", k=nk)
            po, pob = s.bank('po', [4, 5])

            def f():
                for kc in range(nk):
                    ins = nc.tensor.matmul(po[:], Wv[:, kc, :], s.big[:, kc, :], start=(kc == 0), stop=(kc == nk - 1))
                return ins
            S.op('pe', f, reads=[Wb] + s.bigb[0:nk], writes=[pob])
            S.op('act', lambda: nc.scalar.copy(s.ybuf[:, oc, :], po[:]), reads=[pob], writes=[s.ybufb[oc]])
            q, qb = s.T16()
            S.op('act', lambda: nc.scalar.activation(out=q, in_=po[:], func=AF.Square), reads=[pob], writes=[qb])
            S.op('pe', lambda: nc.tensor.matmul(pss[:], s.ones[:], q, start=(oc == 0), stop=(oc == 7)), reads=[qb], writes=[pssb])
        r, rb = s.rstd_from_ss(pss, pssb)
        for oc in range(8):
            S.op('dve', lambda: nc.vector.tensor_tensor(s.ybuf[:, oc, :], s.ybuf[:, oc, :], r, ALU.mult), reads=[rb], writes=[s.ybufb[oc]])
            S.op('dve', lambda: nc.vector.scalar_tensor_tensor(s.xt[:, oc, :], s.ybuf[:, oc, :], GP(oc), s.xt[:, oc, :], ALU.mult, ALU.add),
                 reads=[s.ybufb[oc], s.parb], writes=[s.xtb[oc]])

    def layer(s, b, L):
        nc, S = s.nc, s.S
        src = s.xT[b] if L == 0 else s.xs[(L - 1) % 2]
        dst = s.yT[b] if L == s.NLAYER - 1 else s.xs[L % 2]
        srcv = src.rearrange("(c p) t -> p c t", p=128)
        dstv = dst.rearrange("(c p) t -> p c t", p=128)
        even = (L % 2 == 0)
        if even:
            s.reset_even()
        else:
            S.op('dve', lambda: nc.vector.memset(s.ccar[:], 0.0), writes=[s.ccarb])
        S.op('dve', lambda: nc.vector.memset(s.fcar[:], 0.0), writes=[s.fcarb])
        cf = lambda kind: (lambda c: s.coef[:, L, kind, c, b:b + 1])
        Bm = lambda c: s.modT[:, L, c, b:b + 1]
        Bf = lambda c: s.modT[:, L, 24 + c, b:b + 1]
        for i in range(s.ntile):
            t0 = i * TT
            for c in range(8):
                rd = [] if L == 0 else [s.xsbuf[(L - 1) % 2][i][c]]
                S.dma('pool', s.xt[:, c, :], srcv[:, c, t0:t0 + TT], reads=rd, writes=[s.xtb[c]])
            if s.stage >= 1:
                s.prenorm(cf(0), Bm)
            if even:
                if s.stage >= 2:
                    s.mixer_even(L // 2, i)
                if s.stage >= 7:
                    s.out_proj_residual(16, 'w_out', L // 2, cf(1))
            else:
                if s.stage >= 2:
                    s.mixer_odd(L // 2, i)
                if s.stage >= 7:
                    s.out_proj_residual(8, 'w_sco', L // 2, cf(1))
            if s.stage >= 8:
                s.prenorm(cf(2), Bf)
                s.ffn(L, i)
            if s.stage >= 9:
                s.out_proj_residual(NFC, 'w_dn', L, cf(3))
            for c in range(8):
                if L == s.NLAYER - 1:
                    ob = Buf("yout")
                    s.ybuf_out.append(ob)
                    wr = [ob]
                else:
                    wr = [s.xsbuf[L % 2][i][c]]
                S.dma('pool', dstv[:, c, t0:t0 + TT], s.xt[:, c, :], reads=[s.xtb[c]], writes=wr)

    def ffn(s, L, i):
        nc, S = s.nc, s.S
        pb = PV_FFN + L * 88
        for c in range(NFC):
            W, Wb = s.wload(s.wb['w_gu'][L, c], 128, 2048)
            Wv = W[:, 0:2048].rearrange("p (j k m) -> p j k m", j=2, k=8)
            pg, pgb = s.bank('pp', [0, 1, 2, 3])
            pu, pub = s.bank('pp', [0, 1, 2, 3])
            for jj, (pp_, ppb_) in enumerate(((pg, pgb), (pu, pub))):
                def f():
                    for kc in range(8):
                        ins = nc.tensor.matmul(pp_[:], Wv[:, jj, kc, :], s.hT[:, kc, :], start=(kc == 0), stop=(kc == 7))
                    return ins
                S.op('pe', f, reads=[Wb] + s.hTb, writes=[ppb_])
            ci = s.rotate('cin', [0, 1])
            cin, cinb = s.cin[:, ci, :], s.cinb[ci]
            S.op('dve', lambda: nc.vector.tensor_copy(cin[:, 0:2], s.fcar[:, c, :]), reads=[s.fcarb], writes=[cinb])
            S.op('act', lambda: nc.scalar.copy(cin[:, 2:2 + TT], pg[:]), reads=[pgb], writes=[cinb])
            t, tb = s.T32()
            S.op('act', lambda: nc.scalar.activation(out=t, in_=pg[:], func=AF.Identity, scale=s.pv(pb + c * 3 + 2), bias=s.pv(pb + 66 + c)),
                 reads=[pgb], writes=[tb])
            for k in range(2):
                S.op('dve', lambda: nc.vector.scalar_tensor_tensor(t, cin[:, k:k + TT], s.pv(pb + c * 3 + k), t, ALU.mult, ALU.add),
                     reads=[cinb], writes=[tb])
            S.op('dve', lambda: nc.vector.tensor_copy(s.fcar[:, c, :], cin[:, TT:TT + 2]), reads=[cinb], writes=[s.fcarb])
            S.op('act', lambda: nc.scalar.activation(out=t, in_=t, func=AF.Silu), reads=[tb], writes=[tb])
            S.op('dve', lambda: nc.vector.tensor_tensor(s.big[:, c, :], t, pu[:], ALU.mult), reads=[tb, pub], writes=[s.bigb[c]])

    def mixer_odd(s, o, i):
        nc, S = s.nc, s.S
        pb = PV_ODD + o * 32
        for c in range(8):
            W, Wb = s.wload(s.wb['w_sc'][o, c], 128, 3072)
            Wv = W[:, 0:3072].rearrange("p (j k m) -> p j k m", j=3, k=8)
            banks = [s.bank('pp', [0, 1, 2, 3]) for _ in range(3)]
            for jj in range(3):
                pp_, ppb_ = banks[jj]

                def f():
                    for kc in range(8):
                        ins = nc.tensor.matmul(pp_[:], Wv[:, jj, kc, :], s.hT[:, kc, :], start=(kc == 0), stop=(kc == 7))
                    return ins
                S.op('pe', f, reads=[Wb] + s.hTb, writes=[ppb_])
            (pbg, pbgb), (pcg, pcgb), (pv_, pvb) = banks
            ci = s.rotate('cin', [0, 1])
            cin, cinb = s.cin[:, ci, :], s.cinb[ci]
            t0, t0b = s.T32()
            S.op('act', lambda: nc.scalar.copy(t0, pcg[:]), reads=[pcgb], writes=[t0b])
            S.op('dve', lambda: nc.vector.tensor_copy(cin[:, 0:2], s.ccar[:, c, 0:2]), reads=[s.ccarb], writes=[cinb])
            S.op('dve', lambda: nc.vector.tensor_tensor(cin[:, 2:2 + TT], t0, pv_[:], ALU.mult), reads=[t0b, pvb], writes=[cinb])
            t, tb = s.T32()
            S.op('act', lambda: nc.scalar.activation(out=t, in_=cin[:, 2:2 + TT], func=AF.Identity, scale=s.pv(pb + c * 3 + 2), bias=s.pv(pb + 24 + c)),
                 reads=[cinb], writes=[tb])
            for k in range(2):
                S.op('dve', lambda: nc.vector.scalar_tensor_tensor(t, cin[:, k:k + TT], s.pv(pb + c * 3 + k), t, ALU.mult, ALU.add),
                     reads=[cinb], writes=[tb])
            S.op('dve', lambda: nc.vector.tensor_copy(s.ccar[:, c, 0:2], cin[:, TT:TT + 2]), reads=[cinb], writes=[s.ccarb])
            S.op('dve', lambda: nc.vector.tensor_tensor(s.big[:, c, :], t, pbg[:], ALU.mult), reads=[tb, pbgb], writes=[s.bigb[c]])

    def reset_even(s):
        nc, S = s.nc, s.S
        S.op('dve', lambda: nc.vector.memset(s.ccar[:], 0.0), writes=[s.ccarb])
        S.op('dve', lambda: nc.vector.memset(s.hst[:], 0.0), writes=[s.hstb])
        S.op('dve', lambda: nc.vector.memset(s.hkT[:], 0.0), writes=[s.hkTb])
        S.op('dve', lambda: nc.vector.memset(s.hvT[:], 0.0), writes=[s.hvTb])
        S.op('dve', lambda: nc.vector.memset(s.kcT[:], 0.0), writes=[s.kcTb])
        S.op('dve', lambda: nc.vector.memset(s.vcT[:], 0.0), writes=[s.vcTb])

    def rope_apply(s, pq, pqb, src16, src16b, dst, dstb, bk=('pp', [0, 1, 2, 3])):
        nc, S = s.nc, s.S
        psw, pswb = s.bank(*bk)
        S.op('pe', lambda: nc.tensor.matmul(psw[0:64, :], s.swm[:], src16, start=True, stop=True), reads=[src16b, s.constb], writes=[pswb])
        a, ab = s.T32()
        S.op('dve', lambda: nc.vector.tensor_tensor(a[0:64, :], pq[0:64, :], s.rope[:, 0, :], ALU.mult), reads=[pqb, s.ropeb], writes=[ab])
        b2, b2b = s.T32()
        S.op('dve', lambda: nc.vector.tensor_tensor(b2[0:64, :], psw[0:64, :], s.rope[:, 1, :], ALU.mult), reads=[pswb, s.ropeb], writes=[b2b])
        S.op('pool', lambda: nc.gpsimd.tensor_tensor(dst, a[0:64, :], b2[0:64, :], ALU.add), reads=[ab, b2b], writes=[dstb])

    def mixer_even(s, e, i):
        nc, S = s.nc, s.S
        t0 = i * TT
        pbase = PV_EVEN + e * 64
        S.dma('pool', s.rope[:], s.cd['c_rope'][:, :, t0:t0 + TT], writes=[s.ropeb])
        S.dma('pool', s.cmask[:], s.cd['c_cm'][:, t0:t0 + TT], writes=[s.cmaskb])
        S.dma('pool', s.bon[:].rearrange("p a b -> p (a b)"), s.cd['c_bon'][:, i * 128:(i + 1) * 128], writes=[s.bonb])
        S.dma('sp', s.bdw[:], s.wb['w_bd'][e], writes=[s.bdwb])
        BDb = s.bdwb
        BDv = s.bdw[:].rearrange("p (c j m) -> p c j m", c=8, j=2)
        for c in range(8):
            W, Wb = s.wload(s.wb['w_rnn'][e, c], 128, 2048)
            Wv = W[:, 0:2048].rearrange("p (j k m) -> p j k m", j=2, k=8)
            pxr, pxrb = s.bank('pp', [0, 1, 2, 3])
            pgr, pgrb = s.bank('pp', [0, 1, 2, 3])
            for jj, (pp_, ppb_) in enumerate(((pxr, pxrb), (pgr, pgrb))):
                def f():
                    for kc in range(8):
                        ins = nc.tensor.matmul(pp_[:], Wv[:, jj, kc, :], s.hT[:, kc, :], start=(kc == 0), stop=(kc == 7))
                    return ins
                S.op('pe', f, reads=[Wb] + s.hTb, writes=[ppb_])
            ci = s.rotate('cin', [0, 1])
            cin, cinb = s.cin[:, ci, :], s.cinb[ci]
            S.op('dve', lambda: nc.vector.tensor_copy(cin[:, 0:3], s.ccar[:, c, 0:3]), reads=[s.ccarb], writes=[cinb])
            S.op('act', lambda: nc.scalar.copy(cin[:, 3:3 + TT], pxr[:]), reads=[pxrb], writes=[cinb])
            xc, xcb = s.T32()
            S.op('act', lambda: nc.scalar.activation(out=xc, in_=pxr[:], func=AF.Identity, scale=s.pv(pbase + c * 4 + 3), bias=s.pv(pbase + 32 + c)),
                 reads=[pxrb], writes=[xcb])
            for k in range(3):
                S.op('dve', lambda: nc.vector.scalar_tensor_tensor(xc, cin[:, k:k + TT], s.pv(pbase + c * 4 + k), xc, ALU.mult, ALU.add),
                     reads=[cinb], writes=[xcb])
            S.op('dve', lambda: nc.vector.tensor_copy(s.ccar[:, c, 0:3], cin[:, TT:TT + 3]), reads=[cinb], writes=[s.ccarb])
            x16, x16b = s.T16()
            S.op('pool', lambda: nc.gpsimd.tensor_copy(x16, xc), reads=[xcb], writes=[x16b])
            pr, prb = s.bank('pg', [4, 5])
            pi, pib = s.bank('pg', [4, 5])
            S.op('pe', lambda: nc.tensor.matmul(pr[:], BDv[:, c, 0, :], x16, start=True, stop=True), reads=[BDb, x16b], writes=[prb])
            S.op('pe', lambda: nc.tensor.matmul(pi[:], BDv[:, c, 1, :], x16, start=True, stop=True), reads=[BDb, x16b], writes=[pib])
            ra, rab = s.T32()
            iu, iub = s.T32()
            S.op('act', lambda: nc.scalar.activation(out=ra, in_=pr[:], func=AF.Sigmoid, bias=s.pv(pbase + 40 + c), scale=1.0), reads=[prb], writes=[rab])
            S.op('act', lambda: nc.scalar.activation(out=iu, in_=pi[:], func=AF.Sigmoid, bias=s.pv(pbase + 48 + c), scale=1.0), reads=[pib], writes=[iub])
            S.op('act', lambda: nc.scalar.activation(out=ra, in_=ra, func=AF.Exp, scale=s.cl[:, e, c:c + 1]), reads=[rab, s.parb], writes=[rab])
            S.op('dve', lambda: nc.vector.tensor_tensor(iu, iu, xc, ALU.mult), reads=[iub, xcb], writes=[iub])
            m, mb = s.T32()
            S.op('act', lambda: nc.scalar.activation(out=m, in_=ra, func=AF.Square), reads=[rab], writes=[mb])
            S.op('act', lambda: nc.scalar.activation(out=m, in_=m, func=AF.Sqrt, scale=-1.0, bias=1.0), reads=[mb], writes=[mb])
            S.op('dve', lambda: nc.vector.tensor_tensor(iu, iu, m, ALU.mult), reads=[iub, mb], writes=[iub])
            S.op('dve', lambda: nc.vector.tensor_tensor_scan(m, ra, iu, s.hst[:, c:c + 1], ALU.mult, ALU.add), reads=[rab, iub, s.hstb], writes=[mb])
            S.op('dve', lambda: nc.vector.tensor_copy(s.hst[:, c:c + 1], m[:, TT - 1:TT]), reads=[mb], writes=[s.hstb])
            gg, ggb = s.T32()
            S.op('act', lambda: nc.scalar.activation(out=gg, in_=pgr[:], func=AF.Gelu_apprx_tanh), reads=[pgrb], writes=[ggb])
            S.op('dve', lambda: nc.vector.tensor_tensor(s.big[:, c, :], m, gg, ALU.mult), reads=[mb, ggb], writes=[s.bigb[c]])
        if s.stage < 3:
            return
        Wvs, Wvsb = s.wload(s.wb['w_vs'][e], 128, 2048)
        Wvw, Wvwb = s.wload(s.wb['w_vw'][e], 128, 2048)
        Wgl, Wglb = s.wload(s.wb['w_gl'][e], 128, 384)
        Wvsv = Wvs[:, 0:2048].rearrange("p (k m) -> p k m", k=8)
        Wvwv = Wvw[:, 0:2048].rearrange("p (k m) -> p k m", k=8)
        Wglv = Wgl[:, 0:384].rearrange("p (k m) -> p k m", k=8)
        for j in range(4):
            kt = 4 * i + j
            pv_, pvb = s.bank('pp', [0, 1, 2, 3])
            pg_, pgb_ = s.bank('pp', [0, 1, 2, 3])

            def f():
                for kc in range(8):
                    nc.tensor.matmul(pv_[:, 0:256], s.hT[:, kc, j * 128:(j + 1) * 128], Wvsv[:, kc, :], start=(kc == 0), stop=(kc == 7))
                for kc in range(8):
                    ins = nc.tensor.matmul(pv_[:, 256:512], s.hT[:, kc, j * 128:(j + 1) * 128], Wvwv[:, kc, :], start=(kc == 0), stop=(kc == 7))
                return ins
            S.op('pe', f, reads=[Wvsb, Wvwb] + s.hTb, writes=[pvb])

            def f2():
                for kc in range(8):
                    ins = nc.tensor.matmul(pg_[:, 0:48], s.hT[:, kc, j * 128:(j + 1) * 128], Wglv[:, kc, :], start=(kc == 0), stop=(kc == 7))
                return ins
            S.op('pe', f2, reads=[Wglb] + s.hTb, writes=[pgb_])
            S.op('act', lambda: nc.scalar.copy(s.vsA[:, kt, :, 0:64], pv_[:, 0:256].rearrange("p (g d) -> p g d", g=4)), reads=[pvb], writes=[s.vsAb])
            S.op('act', lambda: nc.scalar.copy(s.vwA[:, kt % 8, :, 0:64], pv_[:, 256:512].rearrange("p (g d) -> p g d", g=4)), reads=[pvb], writes=[s.vwAb])
            S.op('act', lambda: nc.scalar.activation(out=s.gsig[:, j, :], in_=pg_[:, 0:48], func=AF.Sigmoid), reads=[pgb_], writes=[s.gsigb])
        S.op('dve', lambda: nc.vector.tensor_copy(s.kcT[:, :, 0:16], s.kcT[:, :, TT:TT + 16]), reads=[s.kcTb], writes=[s.kcTb])
        S.op('dve', lambda: nc.vector.tensor_copy(s.vcT[:, :, 0:16], s.vcT[:, :, TT:TT + 16]), reads=[s.vcTb], writes=[s.vcTb])
        kvW = []
        for g in range(4):
            W, Wb = s.wload(s.wb['w_kv'][e, g], 128, 2048)
            Wv = W[:, 0:2048].rearrange("p (k t m) -> p k t m", k=8, t=4)
            for ti in range(4):
                pk, pkb = s.bank('pp', [0, 1, 2, 3])

                def f():
                    for kc in range(8):
                        ins = nc.tensor.matmul(pk[0:64, :], Wv[:, kc, ti, :], s.hT[:, kc, :], start=(kc == 0), stop=(kc == 7))
                    return ins
                S.op('pe', f, reads=[Wb] + s.hTb, writes=[pkb])
                if ti < 2:
                    k16, k16b = s.T16()
                    S.op('act', lambda: nc.scalar.copy(k16[0:64, :], pk[0:64, :]), reads=[pkb], writes=[k16b])
                    if ti == 0:
                        s.rope_apply(pk, pkb, k16[0:64, :], k16b, s.ksA[0:64, g, t0:t0 + TT], s.ksAb)
                    else:
                        r0 = (t0 % 1024)
                        s.rope_apply(pk, pkb, k16[0:64, :], k16b, s.kwT[0:64, g, r0:r0 + TT], s.kwTb)
                elif ti == 2:
                    S.op('act', lambda: nc.scalar.copy(s.kcT[:, g, 16:16 + TT], pk[0:64, :]), reads=[pkb], writes=[s.kcTb])
                else:
                    S.op('act', lambda: nc.scalar.copy(s.vcT[:, g, 16:16 + TT], pk[0:64, :]), reads=[pkb], writes=[s.vcTb])
        if s.stage < 4:
            return
        S.dma('sp', s.wcm[:], s.wb['w_cm'][e], writes=[s.wcmb])
        Wcm, Wcmb = s.wcm, s.wcmb
        for kv in range(2):
            srcT, srcTb = (s.kcT, s.kcTb) if kv == 0 else (s.vcT, s.vcTb)
            W1 = []
            for hf in range(2):
                W, Wb = s.wload(s.wb['w_c1'][e, kv, hf], 64, 2048)
                W1.append((W[0:64, 0:2048].rearrange("p (l m) -> p l m", l=16), Wb))
            if i == 0:
                pbi, pbib = s.bank('pp', [0, 1, 2, 3])

                def fb():
                    for l in range(32):
                        Wl, _ = W1[l // 16]
                        c0 = 128 + (kv * 32 + l) * 8
                        ins = nc.tensor.matmul(pbi[:, 0:8], Wl[:, l % 16, :], Wcm[0:64, c0:c0 + 8], start=(l == 0), stop=(l == 31))
                    return ins
                S.op('pe', fb, reads=[W1[0][1], W1[1][1], Wcmb], writes=[pbib])
                S.op('act', lambda: nc.scalar.copy(s.cbias[:, kv:kv + 1], pbi[:, 0:1]), reads=[pbib], writes=[s.cbiasb])
            ph, phb = s.bank('pp', [0, 1, 2, 3])

            def fh():
                for l in range(32):
                    Wl, _ = W1[l // 16]
                    ins = nc.tensor.matmul(ph[:, 0:128], Wl[:, l % 16, :], srcT[:, :, l:l + 16 * 31 + 1:16], start=(l == 0), stop=(l == 31))
                return ins
            S.op('pe', fh, reads=[W1[0][1], W1[1][1], srcTb], writes=[phb])
            hdst, hdstb = (s.hkT, s.hkTb) if kv == 0 else (s.hvT, s.hvTb)
            S.op('act', lambda: nc.scalar.activation(out=hdst[:, :, 32 * i:32 * i + 32], in_=ph[:, 0:128].rearrange("p (g n) -> p g n", g=4),
                                                      func=AF.Gelu_apprx_tanh, bias=s.cbias[:, kv:kv + 1], scale=1.0),
                 reads=[phb, s.cbiasb], writes=[hdstb])
        pkc, pkcb = s.bank('pp', [0, 1, 2, 3])
        S.op('pe', lambda: nc.tensor.matmul(pkc[0:64, :], Wcm[:, 0:64], s.hkT[:].rearrange("p g n -> p (g n)"), start=True, stop=True),
             reads=[Wcmb, s.hkTb], writes=[pkcb])
        S.op('act', lambda: nc.scalar.copy(s.kcmpT[:].rearrange("p g n -> p (g n)"), pkc[0:64, :]), reads=[pkcb], writes=[s.kcmpTb])
        pvc, pvcb = s.bank('pp', [0, 1, 2, 3])

        def fv():
            for g in range(4):
                ins = nc.tensor.matmul(pvc[:, g * 64:(g + 1) * 64], s.hvT[:, g, :], Wcm[:, 64:128], start=True, stop=True, skip_group_check=True)
            return ins
        S.op('pe', fv, reads=[Wcmb, s.hvTb], writes=[pvcb])
        S.op('act', lambda: nc.scalar.copy(s.vcmpA[:, :, 0:64], pvc[:, 0:256].rearrange("p (g d) -> p g d", g=4)), reads=[pvcb], writes=[s.vcmpAb])
        if s.stage < 5:
            return
        for g in range(4):
            W, Wb = s.wload(s.wb['w_q'][e, g], 128, 2048)
            Wv = W[:, 0:2048].rearrange("p (k m) -> p k m", k=8)
            for r in range(4):
                pq, pqb = s.bank('ppa', [0, 1])

                def f():
                    for kc in range(8):
                        ins = nc.tensor.matmul(pq[0:64, :], Wv[:, kc, r * 64:(r + 1) * 64], s.hT[:, kc, :], start=(kc == 0), stop=(kc == 7))
                    return ins
                S.op('pe', f, reads=[Wb] + s.hTb, writes=[pqb])
                S.op('act', lambda: nc.scalar.copy(s.qA[:, r, :], pq[0:64, :]), reads=[pqb], writes=[s.qAb])
                s.rope_apply(pq, pqb, s.qA[:, r, :], s.qAb, s.qrA[0:64, r, :], s.qrAb, bk=('ppa', [0, 1]))
            s.attn_phaseA(g, i)
            if s.stage >= 6:
                s.attn_phaseB(g, i)
        if s.stage < 6:
            return
        for qq in range(4):
            ptr, ptrb = s.ps[7], s.psb[7]
            ptr16 = ptr[:].bitcast(BF16)

            def ft():
                for cc in range(8):
                    ins = nc.tensor.transpose(ptr16[:, cc * 128:(cc + 1) * 128], s.yatt[:, qq, cc * 128:(cc + 1) * 128], s.ident[:])
                return ins
            S.op('pe', ft, reads=[s.yattb[qq], s.constb], writes=[ptrb])
            S.op('act', lambda: nc.scalar.copy(s.big[:, 8:16, qq * 128:(qq + 1) * 128], ptr16[:, 0:1024].rearrange("p (c q) -> p c q", c=8)),
                 reads=[ptrb], writes=s.bigb[8:16])

    def attn_phaseA(s, g, i):
        nc, S = s.nc, s.S
        st = {}

        def A1(qq):
            qs = slice(qq * 128, (qq + 1) * 128)
            psc, pscb = s.bank('sc', [2, 3])

            def f():
                nc.tensor.matmul(psc[:], s.kcmpT[:, g, :], s.qA[:, :, qs], start=True, stop=False)
                return nc.tensor.matmul(psc[:], s.ident[:], s.cmask[:, qs].unsqueeze(1).to_broadcast([128, 4, 128]), start=False, stop=True)
            S.op('pe', f, reads=[s.kcmpTb, s.qAb, s.cmaskb, s.constb], writes=[pscb])
            pc, pcb = s.PT()
            S.op('act', lambda: nc.scalar.activation(out=pc, in_=psc[:], func=AF.Exp, scale=0.125), reads=[pscb], writes=[pcb])
            st[qq] = (pc, pcb)

        def A2(qq):
            qt = 4 * i + qq
            pc, pcb = st[qq]
            poc, pocb = s.bank('poc', [4, 5])

            def f2():
                for r in range(4):
                    ins = nc.tensor.matmul(poc[:, r * 97:(r + 1) * 97], pc[:, r * 128:(r + 1) * 128], s.vcmpA[:, g, 0:97], start=True, stop=True, skip_group_check=True)
                return ins
            S.op('pe', f2, reads=[pcb, s.vcmpAb], writes=[pocb])
            pocv = poc[:, 0:388].rearrange("p (r c) -> p r c", r=4)
            rc, rcb = s.SM()
            S.op('dve', lambda: nc.vector.tensor_scalar(rc, pocv[:, :, 64], 1e-30, None, ALU.max), reads=[pocb], writes=[rcb])
            S.op('dve', lambda: nc.vector.reciprocal(rc, rc), reads=[rcb], writes=[rcb])
            ii = s.rotate('imp', [0, 1])
            imp, impb = s.imp[:, ii, 0:32], s.impb[ii]
            mx8 = s.imp[:, ii, 32:40]
            S.op('dve', lambda: nc.vector.scalar_tensor_tensor(imp, pocv[:, 0, 65:97], rc[:, 0:1], s.bon[:, qq, :], ALU.mult, ALU.add), reads=[pocb, rcb, s.bonb], writes=[impb])
            for r in range(1, 4):
                S.op('dve', lambda: nc.vector.scalar_tensor_tensor(imp, pocv[:, r, 65:97], rc[:, r:r + 1], imp, ALU.mult, ALU.add), reads=[pocb, rcb], writes=[impb])
            S.op('dve', lambda: nc.vector.max(out=mx8, in_=imp), reads=[impb], writes=[impb])
            Mq, Mqb = s.rotate('Mq', [(s.Mq, s.Mqb), (s.Mq2, s.Mq2b)])
            S.op('dve', lambda: nc.vector.tensor_scalar(Mq[:, 64:96], imp, mx8[:, 3:4], MASKV, ALU.is_lt, ALU.mult), reads=[impb], writes=[Mqb])
            st[('m', qq)] = (Mq, Mqb)
            gv = s.gsig[:, qq, g * 12:(g + 1) * 12].rearrange("p (r b) -> p r b", r=4)
            S.op('dve', lambda: nc.vector.tensor_tensor(rc, rc, gv[:, :, 0], ALU.mult), reads=[rcb, s.gsigb], writes=[rcb])
            o1 = s.oc[:, qq, :].rearrange("p (r d) -> p r d", r=4)
            S.op('dve', lambda: nc.vector.tensor_tensor(o1, pocv[:, :, 0:64], rc.unsqueeze(2).to_broadcast([128, 4, 64]), ALU.mult),
                 reads=[pocb, rcb], writes=[s.ocb[qq]])

        def A3(qq):
            qs = slice(qq * 128, (qq + 1) * 128)
            Mq, Mqb = st[('m', qq)]
            ptr, ptrb = s.bank('ptr', [6, 7])
            ptr16 = ptr[:].bitcast(BF16)
            S.op('pe', lambda: nc.tensor.transpose(ptr16[0:96, 0:128], Mq[:, 0:96], s.ident[:]), reads=[Mqb, s.constb], writes=[ptrb])
            S.op('act', lambda: nc.scalar.copy(s.qrA[64:96, :, qs], ptr16[64:96, 0:128].unsqueeze(1).to_broadcast([32, 4, 128])), reads=[ptrb], writes=[s.qrAb])

        for step in range(6):
            if step < 4:
                A1(step)
            if 0 <= step - 1 < 4:
                A2(step - 1)
            if 0 <= step - 2 < 4:
                A3(step - 2)

    def attn_phaseB(s, g, i):
        nc, S = s.nc, s.S
        steps = []
        for qq in range(4):
            qt = 4 * i + qq
            sel = list(range(qt + 1))
            win = [k for k in (qt - 2, qt - 1, qt) if k >= 0]
            for n, kt in enumerate(sel):
                steps.append((qq, 's', kt, n == 0, n == len(sel) - 1))
            for n, kt in enumerate(win):
                steps.append((qq, 'w', kt, n == 0, n == len(win) - 1))

        def score(stp):
            qq, kind, kt, first, last = stp
            qt = 4 * i + qq
            qs = slice(qq * 128, (qq + 1) * 128)
            pss, pssb = s.bank('sc', [2, 3])
            if kind == 's':
                msk = s.mc if kt == qt else None

                def f():
                    ins = nc.tensor.matmul(pss[:], s.ksA[0:96, g, kt * 128:(kt + 1) * 128], s.qrA[0:96, :, qs], start=True, stop=(msk is None))
                    if msk is not None:
                        ins = nc.tensor.matmul(pss[:], s.ident[:], msk[:].unsqueeze(1).to_broadcast([128, 4, 128]), start=False, stop=True)
                    return ins
                S.op('pe', f, reads=[s.ksAb, s.qrAb, s.constb], writes=[pssb])
            else:
                msk = s.mc if kt == qt else (s.mu if kt == qt - 2 else None)
                r0 = (kt % 8) * 128

                def f():
                    ins = nc.tensor.matmul(pss[:], s.kwT[0:64, g, r0:r0 + 128], s.qrA[0:64, :, qs], start=True, stop=(msk is None))
                    if msk is not None:
                        ins = nc.tensor.matmul(pss[:], s.ident[:], msk[:].unsqueeze(1).to_broadcast([128, 4, 128]), start=False, stop=True)
                    return ins
                S.op('pe', f, reads=[s.kwTb, s.qrAb, s.constb], writes=[pssb])
            return pss, pssb

        def finish(stp, pss, pssb):
            qq, kind, kt, first, last = stp
            pt_, ptb_ = s.PT()
            S.op('act', lambda: nc.scalar.activation(out=pt_, in_=pss[:], func=AF.Exp, scale=0.125), reads=[pssb], writes=[ptb_])
            if kind == 's':
                acc, accb = s.ps[4 + qq % 2], s.psb[4 + qq % 2]
                V, Vb = s.vsA[:, kt, g, 0:65], s.vsAb
            else:
                acc, accb = s.ps[6 + qq % 2], s.psb[6 + qq % 2]
                V, Vb = s.vwA[:, kt % 8, g, 0:65], s.vwAb

            def f3():
                for r in range(4):
                    ins = nc.tensor.matmul(acc[:, r * 65:(r + 1) * 65], pt_[:, r * 128:(r + 1) * 128], V,
                                           start=(first and r == 0), stop=(last and r == 3), skip_group_check=True)
                return ins
            S.op('pe', f3, reads=[ptb_, Vb], writes=[accb])
            if kind == 'w' and last:
                combine(qq)

        def combine(qq):
            pos, posb = s.ps[4 + qq % 2], s.psb[4 + qq % 2]
            pow_, powb = s.ps[6 + qq % 2], s.psb[6 + qq % 2]
            posv = pos[:, 0:260].rearrange("p (r c) -> p r c", r=4)
            powv = pow_[:, 0:260].rearrange("p (r c) -> p r c", r=4)
            gv = s.gsig[:, qq, g * 12:(g + 1) * 12].rearrange("p (r b) -> p r b", r=4)
            rs, rsb = s.SM()
            rw, rwb = s.SM()
            S.op('dve', lambda: nc.vector.reciprocal(rs, posv[:, :, 64]), reads=[posb], writes=[rsb])
            S.op('dve', lambda: nc.vector.reciprocal(rw, powv[:, :, 64]), reads=[powb], writes=[rwb])
            S.op('dve', lambda: nc.vector.tensor_tensor(rs, rs, gv[:, :, 1], ALU.mult), reads=[rsb, s.gsigb], writes=[rsb])
            S.op('dve', lambda: nc.vector.tensor_tensor(rw, rw, gv[:, :, 2], ALU.mult), reads=[rwb, s.gsigb], writes=[rwb])
            o1 = s.oc[:, qq, :].rearrange("p (r d) -> p r d", r=4)
            o1b = s.ocb[qq]
            oi2 = 4
            o2 = s.oc[:, oi2, :].rearrange("p (r d) -> p r d", r=4)
            o2b = s.ocb[oi2]
            bc = lambda x: x.unsqueeze(2).to_broadcast([128, 4, 64])
            S.op('dve', lambda: nc.vector.tensor_tensor(o2, posv[:, :, 0:64], bc(rs), ALU.mult), reads=[posb, rsb], writes=[o2b])
            S.op('pool', lambda: nc.gpsimd.tensor_tensor(o1, o1, o2, ALU.add), reads=[o1b, o2b], writes=[o1b])
            S.op('dve', lambda: nc.vector.tensor_tensor(o2, powv[:, :, 0:64], bc(rw), ALU.mult), reads=[powb, rwb], writes=[o2b])
            S.op('pool', lambda: nc.gpsimd.tensor_tensor(s.yatt[:, qq, g * 256:(g + 1) * 256].rearrange("p (r d) -> p r d", r=4), o1, o2, ALU.add),
                 reads=[o1b, o2b], writes=[s.yattb[qq]])

        pending = None
        for stp in steps:
            cur = score(stp)
            if pending is not None:
                finish(*pending)
            pending = (stp,) + cur
        finish(*pending)


_CACHE = {}


def _get_prog(NSEQ, NLAYER, taps=()):
    key = (NSEQ, NLAYER, tuple(taps))
    if key not in _CACHE:
        p = Prog(NSEQ, NLAYER, taps)
        _CACHE[key] = p.build()
    return _CACHE[key]


def make_in_maps(inputs, ncores, nseq):
    hw = host_weights(inputs)
    hc = host_consts()
    x = np.asarray(inputs['x'], dtype=np.float32)
    c = np.asarray(inputs['c'], dtype=np.float32)
    maps = []
    for core in range(ncores):
        b0 = core * nseq
        m = dict(hw)
        m.update(hc)
        m['xT'] = np.ascontiguousarray(x[b0:b0 + nseq].transpose(0, 2, 1))
        cc = c[b0:b0 + nseq]
        m['cT'] = np.ascontiguousarray(cc.reshape(nseq, 8, 128).transpose(2, 1, 0)).reshape(128, 8 * nseq)
        maps.append(m)
    return maps


def kernel(**inputs):
    inputs = {k: np.asarray(v) for k, v in inputs.items()}
    ncores, nseq = 8, 4
    nc = _get_prog(nseq, 4)
    maps = make_in_maps(inputs, ncores, nseq)
    res = run_bass_kernel_spmd(nc, maps, core_ids=list(range(ncores)))
    outs = [np.asarray(r["yT"]).transpose(0, 2, 1) for r in res.results]
    return np.ascontiguousarray(np.concatenate(outs, axis=0).astype(np.float32))
```

```python
import numpy as np
from contextlib import ExitStack
import concourse.bass as bass
import concourse.mybir as mybir
from concourse.bass_utils import run_bass_kernel_spmd

F32 = mybir.dt.float32
BF16 = mybir.dt.bfloat16
AF = mybir.ActivationFunctionType
ALU = mybir.AluOpType
AX = mybir.AxisListType

D = 1024
T = 2048
TT = 512
NTILE = T // TT
DFF = 2816
NFC = DFF // 128
EPS = 1e-6
MASKV = -4000.0
NDSEM = 24
NSLOT = 4
SLOTW = 3072
NPV = 864
PV_GAIN = 0
PV_MODB = 128
PV_EVEN = 320
PV_ODD = 448
PV_FFN = 512


class Buf:
    __slots__ = ("name", "w", "r", "excl")

    def __init__(s, name, excl=False):
        s.name = name
        s.w = None
        s.r = {}
        s.excl = excl


class Sched:
    def __init__(s, nc, es):
        s.nc = nc
        s.eng = {'pe': nc.tensor, 'act': nc.scalar, 'dve': nc.vector, 'pool': nc.gpsimd, 'sp': nc.sync}
        s.sem = {k: es.enter_context(nc.semaphore("s_" + k)) for k in ['pe', 'act', 'dve', 'pool']}
        s.cnt = {k: 0 for k in s.sem}
        s.dsem = [es.enter_context(nc.semaphore(f"d{i}")) for i in range(NDSEM)]
        s.dcnt = [0] * NDSEM
        s.dnext = {'pool': 0, 'sp': 0}
        s.waited = {e: {} for e in s.eng}
        s.nins = 0
        s.nwait = 0

    def _wait(s, e, deps):
        need = {}
        for k, v in deps:
            if v > need.get(k, 0):
                need[k] = v
        for k, v in need.items():
            if k == e and e == 'pe':
                continue
            if s.waited[e].get(k, 0) >= v:
                continue
            semh = s.sem[k] if isinstance(k, str) else s.dsem[k[1]]
            s.eng[e].wait_ge(semh, v)
            s.nwait += 1
            s.waited[e][k] = v

    def deps_for(s, reads, writes):
        d = []
        for b in reads:
            if b.w:
                d.append(b.w)
        for b in writes:
            if b.w:
                d.append(b.w)
            d.extend(b.r.items())
        return d

    def _mark(s, tag, reads, writes):
        k, v = tag
        for b in reads:
            if b.r.get(k, 0) < v:
                b.r[k] = v
        for b in writes:
            b.w = tag
            b.r = {}

    def op(s, e, fn, reads=(), writes=()):
        if any(b.excl for b in reads):
            writes = list(writes) + [b for b in reads if b.excl]
            reads = [b for b in reads if not b.excl]
        s._wait(e, s.deps_for(reads, writes))
        ins = fn()
        s.nins += 1
        s.cnt[e] += 1
        ins.then_inc(s.sem[e], 1)
        s._mark((e, s.cnt[e]), reads, writes)
        return ins

    def dma(s, e, out, in_, reads=(), writes=()):
        h = NDSEM // 2
        j = s.dnext[e]
        s.dnext[e] = (j + 1) % h
        i = j if e == 'pool' else h + j
        deps = s.deps_for(reads, writes)
        if s.dcnt[i] > 0:
            deps.append((('d', i), s.dcnt[i]))
        s._wait(e, deps)
        ins = s.eng[e].dma_start(out=out, in_=in_)
        s.nins += 1
        s.dcnt[i] += 16
        ins.then_inc(s.dsem[i], 16)
        s._mark((('d', i), s.dcnt[i]), reads, writes)
        return ins

    def finish(s, e, bufs):
        s._wait(e, [b.w for b in bufs if b.w])


def _tile_k(w, kc):
    K, N = w.shape
    return w.reshape(kc, 128, N).transpose(1, 0, 2)


def host_weights(inp):
    f = np.float32
    NE, NO = 2, 2
    out = {}
    w_in = inp['ab_w_in']
    w_rnn = np.zeros((NE, 8, 128, 2, 8, 128), f)
    w_q = np.zeros((NE, 4, 128, 8, 256), f)
    w_kv = np.zeros((NE, 4, 128, 8, 4, 64), f)
    w_vs = np.zeros((NE, 128, 8, 256), f)
    w_vw = np.zeros((NE, 128, 8, 256), f)
    w_gl = np.zeros((NE, 128, 8, 48), f)
    w_bd = np.zeros((NE, 128, 8, 2, 128), f)
    w_c1 = np.zeros((NE, 2, 2, 64, 16, 128), f)
    w_cm = np.zeros((NE, 128, 640), f)
    w_out = np.zeros((NE, 8, 128, 16, 128), f)
    for e in range(NE):
        wt = _tile_k(w_in[e], 8)
        for c in range(8):
            w_rnn[e, c, :, 0] = wt[:, :, c * 128:(c + 1) * 128]
            w_rnn[e, c, :, 1] = wt[:, :, 1024 + c * 128:1024 + (c + 1) * 128]
        for g in range(4):
            w_q[e, g] = wt[:, :, 2048 + g * 256:2048 + (g + 1) * 256]
            for ti, off in enumerate((3584, 4096, 3072, 3328)):
                w_kv[e, g, :, :, ti, :] = wt[:, :, off + g * 64:off + (g + 1) * 64]
        w_vs[e] = wt[:, :, 3840:4096]
        w_vw[e] = wt[:, :, 4352:4608]
        w_gl[e] = wt[:, :, 4608:4656]
        for c in range(8):
            for hb in range(2):
                n = 2 * c + hb
                w_bd[e, hb * 64:(hb + 1) * 64, c, 0, hb * 64:(hb + 1) * 64] = inp['lru_w_r'][e, n]
                w_bd[e, hb * 64:(hb + 1) * 64, c, 1, hb * 64:(hb + 1) * 64] = inp['lru_w_i'][e, n]
        for kv, nm in enumerate(('cmp_wk1', 'cmp_wv1')):
            w1 = inp[nm][e].reshape(32, 64, 128)
            for hf in range(2):
                w_c1[e, kv, hf] = w1[hf * 16:(hf + 1) * 16].transpose(1, 0, 2)
        w_cm[e, :, 0:64] = inp['cmp_wk2'][e]
        w_cm[e, :, 64:128] = inp['cmp_wv2'][e]
        for kv, nm in enumerate(('cmp_pe_k', 'cmp_pe_v')):
            pe = inp[nm][e]
            w_cm[e, 0:64, 128 + kv * 256:128 + (kv + 1) * 256] = np.repeat(pe.T[:, :, None], 8, axis=2).reshape(64, 256)
        wo = _tile_k(inp['ab_w_out'][e], 16)
        for oc in range(8):
            w_out[e, oc] = wo[:, :, oc * 128:(oc + 1) * 128]
    out['w_rnn'] = w_rnn.reshape(NE, 8, 128, 2048)
    out['w_q'] = w_q.reshape(NE, 4, 128, 2048)
    out['w_kv'] = w_kv.reshape(NE, 4, 128, 2048)
    out['w_vs'] = w_vs.reshape(NE, 128, 2048)
    out['w_vw'] = w_vw.reshape(NE, 128, 2048)
    out['w_gl'] = w_gl.reshape(NE, 128, 384)
    out['w_bd'] = w_bd.reshape(NE, 128, 2048)
    out['w_c1'] = w_c1.reshape(NE, 2, 2, 64, 2048)
    out['w_cm'] = w_cm
    out['w_out'] = w_out.reshape(NE, 8, 128, 2048)
    w_sc = np.zeros((NO, 8, 128, 3, 8, 128), f)
    w_sco = np.zeros((NO, 8, 128, 8, 128), f)
    for o in range(NO):
        wt = _tile_k(inp['sc_w_in'][o], 8)
        for c in range(8):
            for k3 in range(3):
                w_sc[o, c, :, k3] = wt[:, :, k3 * 1024 + c * 128:k3 * 1024 + (c + 1) * 128]
        wo = _tile_k(inp['sc_w_out'][o], 8)
        for oc in range(8):
            w_sco[o, oc] = wo[:, :, oc * 128:(oc + 1) * 128]
    out['w_sc'] = w_sc.reshape(NO, 8, 128, 3072)
    out['w_sco'] = w_sco.reshape(NO, 8, 128, 1024)
    w_gu = np.zeros((4, NFC, 128, 2, 8, 128), f)
    w_dn = np.zeros((4, 8, 128, NFC, 128), f)
    modw = np.zeros((4, 24, 128, 2, 8, 128), f)
    for L in range(4):
        wg = _tile_k(inp['ffn_w_gate'][L], 8)
        wu = _tile_k(inp['ffn_w_up'][L], 8)
        for c in range(NFC):
            w_gu[L, c, :, 0] = wg[:, :, c * 128:(c + 1) * 128]
            w_gu[L, c, :, 1] = wu[:, :, c * 128:(c + 1) * 128]
        wd = _tile_k(inp['ffn_w_down'][L], NFC)
        for oc in range(8):
            w_dn[L, oc] = wd[:, :, oc * 128:(oc + 1) * 128]
        wm = _tile_k(inp['mod_w'][L], 8)
        for jp in range(24):
            for jj in range(2):
                j = 2 * jp + jj
                modw[L, jp, :, jj] = wm[:, :, j * 128:(j + 1) * 128]
    out['w_gu'] = w_gu.reshape(4, NFC, 128, 2048)
    out['w_dn'] = w_dn.reshape(4, 8, 128, NFC * 128)
    out['modw'] = modw.reshape(4, 24, 128, 2048)
    pv = np.zeros((128, NPV), f)

    def fm(v):
        return v.reshape(8, 128).T

    for kind, nm in enumerate(('norm_mix_pre', 'norm_mix_post', 'norm_ffn_pre', 'norm_ffn_post')):
        for L in range(4):
            pv[:, PV_GAIN + kind * 32 + L * 8:PV_GAIN + kind * 32 + L * 8 + 8] = fm(inp[nm][L])
    for L in range(4):
        pv[:, PV_MODB + L * 48:PV_MODB + (L + 1) * 48] = inp['mod_b'][L].reshape(48, 128).T
    for e in range(NE):
        b = PV_EVEN + e * 64
        cw = inp['ab_conv_w'][e]
        pv[:, b:b + 32] = cw.reshape(4, 8, 128).transpose(2, 1, 0).reshape(128, 32)
        pv[:, b + 32:b + 40] = fm(inp['ab_conv_b'][e])
        pv[:, b + 40:b + 48] = fm(inp['lru_b_r'][e])
        pv[:, b + 48:b + 56] = fm(inp['lru_b_i'][e])
        pv[:, b + 56:b + 64] = fm(inp['lru_lam'][e])
    for o in range(NO):
        b = PV_ODD + o * 32
        cw = inp['sc_conv_w'][o]
        pv[:, b:b + 24] = cw.reshape(3, 8, 128).transpose(2, 1, 0).reshape(128, 24)
        pv[:, b + 24:b + 32] = fm(inp['sc_conv_b'][o])
    for L in range(4):
        b = PV_FFN + L * 88
        cw = inp['ffn_conv_w'][L]
        pv[:, b:b + 66] = cw.reshape(3, NFC, 128).transpose(2, 1, 0).reshape(128, 66)
        pv[:, b + 66:b + 88] = inp['ffn_conv_b'][L].reshape(NFC, 128).T
    out['pvec'] = pv
    return out


def host_consts():
    f = np.float32
    c = {}
    c['c_id'] = np.eye(128, dtype=f)
    kk = np.arange(128)[:, None]
    tt = np.arange(128)[None, :]
    c['c_mc'] = np.where(kk <= tt, 0.0, MASKV).astype(f)
    c['c_mu'] = np.where(kk > tt, 0.0, MASKV).astype(f)
    key = np.arange(T)[None, :]
    c['c_E'] = (key // 64 == np.arange(32)[:, None]).astype(f)
    j = np.arange(128)[:, None]
    t = np.arange(T)[None, :]
    c['c_cm'] = np.where((j >= 1) & (16 * j + 15 <= t), 0.0, MASKV).astype(f)
    tabs = (np.arange(16)[None, :, None] * 128 + np.arange(128)[:, None, None])
    jb = np.arange(32)[None, None, :]
    forced = (jb == 0) | (jb == tabs // 64)
    valid = jb * 64 <= tabs
    bon = np.where(valid, np.where(forced, 1.0e4, 0.0), -1.0e30).astype(f)
    c['c_bon'] = bon.reshape(128, 512)
    n = np.arange(128)[:, None] - 1
    jb2 = np.arange(32)[None, :]
    cov = ((n >= 0) & (16 * n < 64 * jb2 + 64) & (16 * n + 32 > 64 * jb2)).astype(f)
    c['c_cov'] = cov
    half = 8
    inv = np.power(np.float32(500000.0), -np.arange(half, dtype=f) / half).astype(f)
    ang = (np.arange(T, dtype=f)[None, :] * inv[:, None]).astype(f)
    cos = np.cos(ang).astype(f)
    sin = np.sin(ang).astype(f)
    rope = np.zeros((64, 2, T), f)
    rope[:, 0, :] = 1.0
    rope[0:8, 0, :] = cos
    rope[8:16, 0, :] = cos
    rope[0:8, 1, :] = -sin
    rope[8:16, 1, :] = sin
    c['c_rope'] = rope
    sw = np.zeros((64, 64), f)
    for m in range(8):
        sw[m + 8, m] = 1.0
        sw[m, m + 8] = 1.0
    c['c_sw'] = sw
    c['c_ones'] = np.ones((128, 128), f)
    return c


WSHAPES = {
    'w_rnn': (2, 8, 128, 2048), 'w_q': (2, 4, 128, 2048), 'w_kv': (2, 4, 128, 2048),
    'w_vs': (2, 128, 2048), 'w_vw': (2, 128, 2048), 'w_gl': (2, 128, 384), 'w_bd': (2, 128, 2048),
    'w_c1': (2, 2, 2, 64, 2048), 'w_cm': (2, 128, 640), 'w_out': (2, 8, 128, 2048),
    'w_sc': (2, 8, 128, 3072), 'w_sco': (2, 8, 128, 1024), 'w_gu': (4, NFC, 128, 2048),
    'w_dn': (4, 8, 128, NFC * 128),
}
CSHAPES = {
    'c_id': (128, 128), 'c_mc': (128, 128), 'c_mu': (128, 128), 'c_E': (32, T), 'c_cm': (128, T),
    'c_bon': (128, 512), 'c_cov': (128, 32), 'c_rope': (64, 2, T), 'c_sw': (64, 64), 'c_ones': (128, 128),
}


class Prog:
    def __init__(s, NSEQ, NLAYER, taps=()):
        s.NSEQ = NSEQ
        s.NLAYER = NLAYER
        s.taps = set(taps)
        s.tapbufs = []
        import os as _os
        s.stage = int(_os.environ.get("KSTAGE", "99"))
        s.ntile = int(_os.environ.get("KTILES", str(NTILE)))
        s.nc = bass.Bass("TRN2", target_bir_lowering=False)
        s.es = ExitStack()

    def sb(s, name, shape, dt):
        return s.es.enter_context(s.nc.sbuf_tensor(name, list(shape), dt))

    def din(s, name, shape, dt=F32):
        return s.nc.dram_tensor(name, list(shape), dt, kind="ExternalInput").ap()

    def tap(s, name, ap, buf, shape):
        if name not in s.taps:
            return
        o = s.nc.dram_tensor("tap_" + name, list(shape), F32, kind="ExternalOutput").ap()
        b = Buf("tap_" + name)
        s.S.dma('pool', o, ap, reads=[buf], writes=[b])
        s.tapbufs.append(b)

    def build(s):
        nc = s.nc
        NSEQ, NLAYER = s.NSEQ, s.NLAYER
        with s.es:
            s.S = S = Sched(nc, s.es)
            s.xT = s.din("xT", [NSEQ, D, T])
            s.cT = s.din("cT", [128, 8 * NSEQ])
            s.pvec_d = s.din("pvec", [128, NPV])
            s.modw_d = s.din("modw", [4, 24, 128, 2048])
            s.wd = {k: s.din(k, sh) for k, sh in WSHAPES.items()}
            s.wb = {k: nc.dram_tensor(k + "_b", list(sh), BF16, kind="Internal").ap() for k, sh in WSHAPES.items()}
            s.wbbuf = {k: Buf(k + "_b") for k in WSHAPES}
            s.cd = {k: s.din(k, sh) for k, sh in CSHAPES.items()}
            s.yT = nc.dram_tensor("yT", [NSEQ, D, T], F32, kind="ExternalOutput").ap()
            s.xs = [nc.dram_tensor(f"xs{i}", [D, T], F32, kind="Internal").ap() for i in range(2)]
            s.xsbuf = [[[Buf(f"xs{i}_{t}_{c}") for c in range(8)] for t in range(NTILE)] for i in range(2)]
            s.ybuf_out = []
            s.ps = [s.es.enter_context(nc.psum_tensor(f"ps{i}", [128, 512], F32)) for i in range(8)]
            s.psb = [Buf(f"ps{i}", excl=True) for i in range(8)]
            s.rot = {}
            s.alloc_sbuf()
            s.prepass()
            for b in range(NSEQ):
                for L in range(NLAYER):
                    s.layer(b, L)
            S.finish('pool', s.ybuf_out + s.tapbufs)
        return nc

    def rotate(s, key, items):
        i = s.rot.get(key, 0)
        s.rot[key] = i + 1
        return items[i % len(items)]

    def bank(s, key, ids):
        i = s.rotate(key, ids)
        return s.ps[i], s.psb[i]

    def alloc_sbuf(s):
        sb = s.sb
        s.xt = sb("xt", [128, 8, TT], F32)
        s.xtb = [Buf(f"xt{c}") for c in range(8)]
        s.hT = sb("hT", [128, 8, TT], BF16)
        s.hTb = [Buf(f"hT{c}") for c in range(8)]
        s.big = sb("big", [128, NFC, TT], BF16)
        s.bigb = [Buf(f"big{c}") for c in range(NFC)]
        s.ybuf = sb("ybuf", [128, 8, TT], F32)
        s.ybufb = [Buf(f"ybuf{c}") for c in range(8)]
        s.tmp = sb("tmp", [128, 5, TT], F32)
        s.tmpb = [Buf(f"tmp{c}") for c in range(5)]
        s.xcd = sb("xcd", [128, 2, TT], F32)
        s.xcdb = [Buf("xcd0"), Buf("xcd1")]
        s.tb16 = sb("tb16", [128, 4, TT], BF16)
        s.tb16b = [Buf(f"tb16_{c}") for c in range(4)]
        s.cin = sb("cin", [128, 2, TT + 4], F32)
        s.cinb = [Buf("cin0"), Buf("cin1")]
        s.pt = sb("pt", [128, 4, TT], BF16)
        s.ptb = [Buf(f"pt{c}") for c in range(4)]
        s.ring = sb("ring", [128, NSLOT, SLOTW], BF16)
        s.ringb = [Buf(f"ring{c}") for c in range(NSLOT)]
        s.ksA = sb("ksA", [128, 4, T], BF16)
        s.ksAb = Buf("ksA")
        s.kwT = sb("kwT", [64, 4, 1024], BF16)
        s.kwTb = Buf("kwT")
        s.vsA = sb("vsA", [128, 16, 4, 66], BF16)
        s.vsAb = Buf("vsA")
        s.vwA = sb("vwA", [128, 8, 4, 66], BF16)
        s.vwAb = Buf("vwA")
        s.hkT = sb("hkT", [128, 4, 128], BF16)
        s.hkTb = Buf("hkT")
        s.hvT = sb("hvT", [128, 4, 128], BF16)
        s.hvTb = Buf("hvT")
        s.kcmpT = sb("kcmpT", [64, 4, 128], BF16)
        s.kcmpTb = Buf("kcmpT")
        s.vcmpA = sb("vcmpA", [128, 4, 98], BF16)
        s.vcmpAb = Buf("vcmpA")
        s.kcT = sb("kcT", [64, 4, 528], BF16)
        s.kcTb = Buf("kcT")
        s.vcT = sb("vcT", [64, 4, 528], BF16)
        s.vcTb = Buf("vcT")
        s.qA = sb("qA", [64, 4, TT], BF16)
        s.qAb = Buf("qA")
        s.qrA = sb("qrA", [96, 4, TT], BF16)
        s.qrAb = Buf("qrA")
        s.yatt = sb("yatt", [128, 4, 1024], BF16)
        s.yattb = [Buf(f"yatt{c}") for c in range(4)]
        s.gsig = sb("gsig", [128, 4, 48], F32)
        s.gsigb = Buf("gsig")
        s.rope = sb("rope", [64, 2, TT], F32)
        s.ropeb = Buf("rope")
        s.cmask = sb("cmask", [128, TT], BF16)
        s.cmaskb = Buf("cmask")
        s.bon = sb("bon", [128, 4, 32], F32)
        s.bonb = Buf("bon")
        s.ident = sb("ident", [128, 128], BF16)
        s.ones = sb("ones", [128, 128], BF16)
        s.mc = sb("mc", [128, 128], BF16)
        s.mu = sb("mu", [128, 128], BF16)
        s.swm = sb("swm", [64, 64], BF16)
        s.constb = Buf("consts")
        s.Mq = sb("Mq", [128, 96], BF16)
        s.Mqb = Buf("Mq")
        s.pvec = sb("pvec_s", [128, NPV], F32)
        s.modT = sb("modT", [128, 4, 48, s.NSEQ], F32)
        s.coef = sb("coef", [128, 4, 4, 8, s.NSEQ], F32)
        s.cl = sb("cl", [128, 2, 8], F32)
        s.parb = Buf("params")
        s.cact = sb("cact", [128, 8, 8], BF16)
        s.ccar = sb("ccar", [128, 8, 4], F32)
        s.ccarb = Buf("ccar")
        s.hst = sb("hst", [128, 8], F32)
        s.hstb = Buf("hst")
        s.fcar = sb("fcar", [128, NFC, 2], F32)
        s.fcarb = Buf("fcar")
        s.cbias = sb("cbias", [128, 2], F32)
        s.cbiasb = Buf("cbias")
        s.sm = sb("sm", [128, 8, 4], F32)
        s.smb = [Buf(f"sm{c}") for c in range(8)]
        s.imp = sb("imp", [128, 2, 40], F32)
        s.impb = [Buf("imp0"), Buf("imp1")]
        s.oc = sb("oc", [128, 5, 256], F32)
        s.ocb = [Buf(f"oc{c}") for c in range(5)]
        s.bdw = sb("bdw", [128, 2048], BF16)
        s.bdwb = Buf("bdw")
        s.wcm = sb("wcm", [128, 640], BF16)
        s.wcmb = Buf("wcm")
        s.rstd = sb("rstd", [128, TT], F32)
        s.rstdb = Buf("rstd")
        s.Mq2 = sb("Mq2", [128, 96], BF16)
        s.Mq2b = Buf("Mq2")

    def T32(s):
        i = s.rotate('tmp', list(range(5)))
        return s.tmp[:, i, :], s.tmpb[i]

    def T16(s):
        i = s.rotate('tb16', list(range(4)))
        return s.tb16[:, i, :], s.tb16b[i]

    def PT(s):
        i = s.rotate('pt', list(range(4)))
        return s.pt[:, i, :], s.ptb[i]

    def SM(s):
        i = s.rotate('sm', list(range(8)))
        return s.sm[:, i, :], s.smb[i]

    def wload(s, src, P, n, srcbuf=None, eng='sp'):
        i = s.rotate('ring', list(range(NSLOT)))
        dst = s.ring[0:P, i, 0:n]
        s.S.dma(eng, dst, src, reads=[srcbuf] if srcbuf is not None else [], writes=[s.ringb[i]])
        return s.ring[:, i, :], s.ringb[i]

    def pv(s, off, n=1):
        return s.pvec[:, off:off + n]

    def prepass(s):
        nc, S = s.nc, s.S
        NSEQ = s.NSEQ
        for k, sh in WSHAPES.items():
            src, dst = s.wd[k], s.wb[k]
            n0 = sh[0]
            for i0 in range(n0):
                if len(sh) >= 4:
                    for i1 in range(0, sh[1], 8):
                        i2 = min(i1 + 8, sh[1])
                        S.dma('pool', dst[i0, i1:i2], src[i0, i1:i2], writes=[])
                else:
                    S.dma('pool', dst[i0], src[i0], writes=[])
        allw = [(('d', i), S.dcnt[i]) for i in range(NDSEM) if S.dcnt[i] > 0]
        S._wait('sp', allw)
        S._wait('pool', allw)
        cb = s.constb
        S.dma('pool', s.ident[:], s.cd['c_id'], writes=[cb])
        S.dma('pool', s.ones[:], s.cd['c_ones'], writes=[])
        S.dma('pool', s.mc[:], s.cd['c_mc'], writes=[])
        S.dma('pool', s.mu[:], s.cd['c_mu'], writes=[])
        S.dma('pool', s.swm[:], s.cd['c_sw'], writes=[])
        S.dma('pool', s.pvec[:], s.pvec_d, writes=[])
        ct = s.sb("ct", [128, 8 * NSEQ], F32)
        S.dma('pool', ct[:], s.cT, writes=[])
        for g in range(4):
            S.dma('pool', s.ksA[64:96, g, :], s.cd['c_E'], writes=[])
            S.dma('pool', s.vcmpA[:, g, 65:97], s.cd['c_cov'], writes=[])
        allw = [(('d', i), S.dcnt[i]) for i in range(NDSEM) if S.dcnt[i] > 0]
        for e in ('pe', 'act', 'dve', 'pool'):
            S._wait(e, allw)
        S.op('dve', lambda: nc.vector.memset(s.vsA[:, :, :, 64:65], 1.0), writes=[s.vsAb])
        S.op('dve', lambda: nc.vector.memset(s.vwA[:, :, :, 64:65], 1.0), writes=[s.vwAb])
        S.op('dve', lambda: nc.vector.memset(s.vcmpA[:, :, 64:65], 1.0), writes=[s.vcmpAb])
        S.op('dve', lambda: nc.vector.memset(s.Mq[:], 0.0), writes=[s.Mqb])
        S.op('dve', lambda: nc.vector.memset(s.Mq2[:], 0.0), writes=[s.Mq2b])
        S.op('dve', lambda: nc.vector.memset(s.kwT[:], 0.0), writes=[s.kwTb])
        S.op('dve', lambda: nc.vector.memset(s.vwA[:, :, :, 0:64], 0.0), writes=[s.vwAb])
        pb = s.parb
        S.op('dve', lambda: nc.vector.memset(s.cact[:], 0.0), writes=[pb])
        S.op('act', lambda: nc.scalar.activation(out=s.cact[:, :, 0:NSEQ], in_=ct[:].rearrange("p (a b) -> p a b", a=8), func=AF.Silu), reads=[pb], writes=[pb])
        for e in range(2):
            lam = s.pv(PV_EVEN + e * 64 + 56, 8)
            S.op('act', lambda: nc.scalar.activation(out=s.cl[:, e, :], in_=lam, func=AF.Exp, scale=-1.0), writes=[pb])
            S.op('act', lambda: nc.scalar.activation(out=s.cl[:, e, :], in_=s.cl[:, e, :], func=AF.Ln, bias=1.0), writes=[pb])
            S.op('act', lambda: nc.scalar.mul(s.cl[:, e, :], s.cl[:, e, :], -8.0), writes=[pb])
        for L in range(s.NLAYER):
            for jp in range(24):
                W, Wb = s.wload(s.modw_d[L, jp], 128, 2048, eng='pool')
                Wv = W[:, 0:2048].rearrange("p (j k m) -> p j k m", j=2, k=8)
                for jj in range(2):
                    j = 2 * jp + jj
                    pm, pmb = s.bank('pp', [0, 1, 2, 3])

                    def f():
                        for kc in range(8):
                            ins = nc.tensor.matmul(pm[:, 0:8], Wv[:, jj, kc, :], s.cact[:, kc, :], start=(kc == 0), stop=(kc == 7))
                        return ins
                    S.op('pe', f, reads=[Wb, pb], writes=[pmb])
                    S.op('act', lambda: nc.scalar.activation(out=s.modT[:, L, j, :], in_=pm[:, 0:NSEQ], func=AF.Identity,
                                                              bias=s.pv(PV_MODB + L * 48 + j), scale=1.0), reads=[pmb], writes=[pb])
            for kind, (gk, j0) in enumerate(((0, 8), (1, 16), (2, 32), (3, 40))):
                gain = s.pvec[:, PV_GAIN + gk * 32 + L * 8:PV_GAIN + gk * 32 + L * 8 + 8]
                S.op('dve', lambda: nc.vector.scalar_tensor_tensor(
                    s.coef[:, L, kind, :, :], s.modT[:, L, j0:j0 + 8, :], 1.0,
                    gain.unsqueeze(2).to_broadcast([128, 8, NSEQ]), ALU.add, ALU.mult), reads=[pb], writes=[pb])

    def rstd_from_ss(s, pss, pssb):
        nc, S = s.nc, s.S
        r, rb = s.rstd[:], s.rstdb
        S.op('act', lambda: nc.scalar.activation(out=r, in_=pss[:], func=AF.Sqrt, scale=1.0 / D, bias=EPS), reads=[pssb], writes=[rb])
        S.op('dve', lambda: nc.vector.reciprocal(r, r), reads=[rb], writes=[rb])
        return r, rb

    def prenorm(s, A, B):
        nc, S = s.nc, s.S
        pss, pssb = s.ps[6], s.psb[6]
        for c in range(8):
            q, qb = s.T16()
            S.op('act', lambda: nc.scalar.activation(out=q, in_=s.xt[:, c, :], func=AF.Square), reads=[s.xtb[c]], writes=[qb])
            S.op('pe', lambda: nc.tensor.matmul(pss[:], s.ones[:], q, start=(c == 0), stop=(c == 7)), reads=[qb, s.constb], writes=[pssb])
        r, rb = s.rstd_from_ss(pss, pssb)
        for c in range(8):
            t, tb = s.T32()
            S.op('dve', lambda: nc.vector.tensor_tensor(t, s.xt[:, c, :], r, ALU.mult), reads=[s.xtb[c], rb], writes=[tb])
            S.op('act', lambda: nc.scalar.activation(out=s.hT[:, c, :], in_=t, func=AF.Identity, scale=A(c), bias=B(c)),
                 reads=[tb, s.parb], writes=[s.hTb[c]])

    def out_proj_residual(s, nk, wfam, widx, GP):
        nc, S = s.nc, s.S
        pss, pssb = s.ps[6], s.psb[6]
        for oc in range(8):
            W, Wb = s.wload(s.wb[wfam][widx][oc], 128, nk * 128, srcbuf=None)
            Wv = W[:, 0:nk * 128].rearrange("p (k m) -> p k m", k=nk)
            po, pob = s.bank('po', [4, 5])

            def f():
                for kc in range(nk):
                    ins = nc.tensor.matmul(po[:], Wv[:, kc, :], s.big[:, kc, :], start=(kc == 0), stop=(kc == nk - 1))
                return ins
            S.op('pe', f, reads=[Wb] + s.bigb[0:nk], writes=[pob])
            S.op('act', lambda: nc.scalar.copy(s.ybuf[:, oc, :], po[:]), reads=[pob], writes=[s.ybufb[oc]])
            q, qb = s.T16()
            S.op('act', lambda: nc.scalar.activation(out=q, in_=po[:], func=AF.Square), reads=[pob], writes=[qb])
            S.op('pe', lambda: nc.tensor.matmul(pss[:], s.ones[:], q, start=(oc == 0), stop=(oc == 7)), reads=[qb], writes=[pssb])
        r, rb = s.rstd_from_ss(pss, pssb)
        for oc in range(8):
            S.op('dve', lambda: nc.vector.tensor_tensor(s.ybuf[:, oc, :], s.ybuf[:, oc, :], r, ALU.mult), reads=[rb], writes=[s.ybufb[oc]])
            S.op('dve', lambda: nc.vector.scalar_tensor_tensor(s.xt[:, oc, :], s.ybuf[:, oc, :], GP(oc), s.xt[:, oc, :], ALU.mult, ALU.add),
                 reads=[s.ybufb[oc], s.parb], writes=[s.xtb[oc]])

    def layer(s, b, L):
        nc, S = s.nc, s.S
        src = s.xT[b] if L == 0 else s.xs[(L - 1) % 2]
        dst = s.yT[b] if L == s.NLAYER - 1 else s.xs[L % 2]
        srcv = src.rearrange("(c p) t -> p c t", p=128)
        dstv = dst.rearrange("(c p) t -> p c t", p=128)
        even = (L % 2 == 0)
        if even:
            s.reset_even()
        else:
            S.op('dve', lambda: nc.vector.memset(s.ccar[:], 0.0), writes=[s.ccarb])
        S.op('dve', lambda: nc.vector.memset(s.fcar[:], 0.0), writes=[s.fcarb])
        cf = lambda kind: (lambda c: s.coef[:, L, kind, c, b:b + 1])
        Bm = lambda c: s.modT[:, L, c, b:b + 1]
        Bf = lambda c: s.modT[:, L, 24 + c, b:b + 1]
        for i in range(s.ntile):
            t0 = i * TT
            for c in range(8):
                rd = [] if L == 0 else [s.xsbuf[(L - 1) % 2][i][c]]
                S.dma('pool', s.xt[:, c, :], srcv[:, c, t0:t0 + TT], reads=rd, writes=[s.xtb[c]])
            if s.stage >= 1:
                s.prenorm(cf(0), Bm)
            if even:
                if s.stage >= 2:
                    s.mixer_even(L // 2, i)
                if s.stage >= 7:
                    s.out_proj_residual(16, 'w_out', L // 2, cf(1))
            else:
                if s.stage >= 2:
                    s.mixer_odd(L // 2, i)
                if s.stage >= 7:
                    s.out_proj_residual(8, 'w_sco', L // 2, cf(1))
            if s.stage >= 8:
                s.prenorm(cf(2), Bf)
                s.ffn(L, i)
            if s.stage >= 9:
                s.out_proj_residual(NFC, 'w_dn', L, cf(3))
            for c in range(8):
                if L == s.NLAYER - 1:
                    ob = Buf("yout")
                    s.ybuf_out.append(ob)
                    wr = [ob]
                else:
                    wr = [s.xsbuf[L % 2][i][c]]
                S.dma('pool', dstv[:, c, t0:t0 + TT], s.xt[:, c, :], reads=[s.xtb[c]], writes=wr)

    def ffn(s, L, i):
        nc, S = s.nc, s.S
        pb = PV_FFN + L * 88
        for c in range(NFC):
            W, Wb = s.wload(s.wb['w_gu'][L, c], 128, 2048)
            Wv = W[:, 0:2048].rearrange("p (j k m) -> p j k m", j=2, k=8)
            pg, pgb = s.bank('ppf', [0, 1, 2, 3, 4, 5, 7])
            pu, pub = s.bank('ppf', [0, 1, 2, 3, 4, 5, 7])
            for jj, (pp_, ppb_) in enumerate(((pg, pgb), (pu, pub))):
                def f():
                    for kc in range(8):
                        ins = nc.tensor.matmul(pp_[:], Wv[:, jj, kc, :], s.hT[:, kc, :], start=(kc == 0), stop=(kc == 7))
                    return ins
                S.op('pe', f, reads=[Wb] + s.hTb, writes=[ppb_])
            ci = s.rotate('cin', [0, 1])
            cin, cinb = s.cin[:, ci, :], s.cinb[ci]
            S.op('dve', lambda: nc.vector.tensor_copy(cin[:, 0:2], s.fcar[:, c, :]), reads=[s.fcarb], writes=[cinb])
            S.op('act', lambda: nc.scalar.copy(cin[:, 2:2 + TT], pg[:]), reads=[pgb], writes=[cinb])
            t, tb = s.T32()
            S.op('act', lambda: nc.scalar.activation(out=t, in_=pg[:], func=AF.Identity, scale=s.pv(pb + c * 3 + 2), bias=s.pv(pb + 66 + c)),
                 reads=[pgb], writes=[tb])
            for k in range(2):
                S.op('dve', lambda: nc.vector.scalar_tensor_tensor(t, cin[:, k:k + TT], s.pv(pb + c * 3 + k), t, ALU.mult, ALU.add),
                     reads=[cinb], writes=[tb])
            S.op('dve', lambda: nc.vector.tensor_copy(s.fcar[:, c, :], cin[:, TT:TT + 2]), reads=[cinb], writes=[s.fcarb])
            S.op('act', lambda: nc.scalar.activation(out=t, in_=t, func=AF.Silu), reads=[tb], writes=[tb])
            S.op('dve', lambda: nc.vector.tensor_tensor(s.big[:, c, :], t, pu[:], ALU.mult), reads=[tb, pub], writes=[s.bigb[c]])

    def mixer_odd(s, o, i):
        nc, S = s.nc, s.S
        pb = PV_ODD + o * 32
        for c in range(8):
            W, Wb = s.wload(s.wb['w_sc'][o, c], 128, 3072)
            Wv = W[:, 0:3072].rearrange("p (j k m) -> p j k m", j=3, k=8)
            banks = [s.bank('ppf', [0, 1, 2, 3, 4, 5, 7]) for _ in range(3)]
            for jj in range(3):
                pp_, ppb_ = banks[jj]

                def f():
                    for kc in range(8):
                        ins = nc.tensor.matmul(pp_[:], Wv[:, jj, kc, :], s.hT[:, kc, :], start=(kc == 0), stop=(kc == 7))
                    return ins
                S.op('pe', f, reads=[Wb] + s.hTb, writes=[ppb_])
            (pbg, pbgb), (pcg, pcgb), (pv_, pvb) = banks
            ci = s.rotate('cin', [0, 1])
            cin, cinb = s.cin[:, ci, :], s.cinb[ci]
            t0, t0b = s.T32()
            S.op('act', lambda: nc.scalar.copy(t0, pcg[:]), reads=[pcgb], writes=[t0b])
            S.op('dve', lambda: nc.vector.tensor_copy(cin[:, 0:2], s.ccar[:, c, 0:2]), reads=[s.ccarb], writes=[cinb])
            S.op('dve', lambda: nc.vector.tensor_tensor(cin[:, 2:2 + TT], t0, pv_[:], ALU.mult), reads=[t0b, pvb], writes=[cinb])
            t, tb = s.T32()
            S.op('act', lambda: nc.scalar.activation(out=t, in_=cin[:, 2:2 + TT], func=AF.Identity, scale=s.pv(pb + c * 3 + 2), bias=s.pv(pb + 24 + c)),
                 reads=[cinb], writes=[tb])
            for k in range(2):
                S.op('dve', lambda: nc.vector.scalar_tensor_tensor(t, cin[:, k:k + TT], s.pv(pb + c * 3 + k), t, ALU.mult, ALU.add),
                     reads=[cinb], writes=[tb])
            S.op('dve', lambda: nc.vector.tensor_copy(s.ccar[:, c, 0:2], cin[:, TT:TT + 2]), reads=[cinb], writes=[s.ccarb])
            S.op('dve', lambda: nc.vector.tensor_tensor(s.big[:, c, :], t, pbg[:], ALU.mult), reads=[tb, pbgb], writes=[s.bigb[c]])

    def reset_even(s):
        nc, S = s.nc, s.S
        S.op('dve', lambda: nc.vector.memset(s.ccar[:], 0.0), writes=[s.ccarb])
        S.op('dve', lambda: nc.vector.memset(s.hst[:], 0.0), writes=[s.hstb])
        S.op('dve', lambda: nc.vector.memset(s.hkT[:], 0.0), writes=[s.hkTb])
        S.op('dve', lambda: nc.vector.memset(s.hvT[:], 0.0), writes=[s.hvTb])
        S.op('dve', lambda: nc.vector.memset(s.kcT[:], 0.0), writes=[s.kcTb])
        S.op('dve', lambda: nc.vector.memset(s.vcT[:], 0.0), writes=[s.vcTb])

    def rope_apply(s, pq, pqb, src16, src16b, dst, dstb, bk=('pp', [0, 1, 2, 3])):
        nc, S = s.nc, s.S
        psw, pswb = s.bank(*bk)
        S.op('pe', lambda: nc.tensor.matmul(psw[0:64, :], s.swm[:], src16, start=True, stop=True), reads=[src16b, s.constb], writes=[pswb])
        a, ab = s.T32()
        S.op('dve', lambda: nc.vector.tensor_tensor(a[0:64, :], pq[0:64, :], s.rope[:, 0, :], ALU.mult), reads=[pqb, s.ropeb], writes=[ab])
        b2, b2b = s.T32()
        S.op('dve', lambda: nc.vector.tensor_tensor(b2[0:64, :], psw[0:64, :], s.rope[:, 1, :], ALU.mult), reads=[pswb, s.ropeb], writes=[b2b])
        S.op('pool', lambda: nc.gpsimd.tensor_tensor(dst, a[0:64, :], b2[0:64, :], ALU.add), reads=[ab, b2b], writes=[dstb])

    def mixer_even(s, e, i):
        nc, S = s.nc, s.S
        t0 = i * TT
        pbase = PV_EVEN + e * 64
        S.dma('pool', s.rope[:], s.cd['c_rope'][:, :, t0:t0 + TT], writes=[s.ropeb])
        S.dma('pool', s.cmask[:], s.cd['c_cm'][:, t0:t0 + TT], writes=[s.cmaskb])
        S.dma('pool', s.bon[:].rearrange("p a b -> p (a b)"), s.cd['c_bon'][:, i * 128:(i + 1) * 128], writes=[s.bonb])
        S.dma('sp', s.bdw[:], s.wb['w_bd'][e], writes=[s.bdwb])
        BDb = s.bdwb
        BDv = s.bdw[:].rearrange("p (c j m) -> p c j m", c=8, j=2)
        rst = {}

        def R1(c):
            W, Wb = s.wload(s.wb['w_rnn'][e, c], 128, 2048)
            Wv = W[:, 0:2048].rearrange("p (j k m) -> p j k m", j=2, k=8)
            pxr, pxrb = s.bank('pp', [0, 1, 2, 3])
            pgr, pgrb = s.bank('pp', [0, 1, 2, 3])
            for jj, (pp_, ppb_) in enumerate(((pxr, pxrb), (pgr, pgrb))):
                def f():
                    for kc in range(8):
                        ins = nc.tensor.matmul(pp_[:], Wv[:, jj, kc, :], s.hT[:, kc, :], start=(kc == 0), stop=(kc == 7))
                    return ins
                S.op('pe', f, reads=[Wb] + s.hTb, writes=[ppb_])
            ci = s.rotate('cin', [0, 1])
            cin, cinb = s.cin[:, ci, :], s.cinb[ci]
            S.op('dve', lambda: nc.vector.tensor_copy(cin[:, 0:3], s.ccar[:, c, 0:3]), reads=[s.ccarb], writes=[cinb])
            S.op('act', lambda: nc.scalar.copy(cin[:, 3:3 + TT], pxr[:]), reads=[pxrb], writes=[cinb])
            xi = s.rotate('xcd', [0, 1])
            xc, xcb = s.xcd[:, xi, :], s.xcdb[xi]
            S.op('act', lambda: nc.scalar.activation(out=xc, in_=pxr[:], func=AF.Identity, scale=s.pv(pbase + c * 4 + 3), bias=s.pv(pbase + 32 + c)),
                 reads=[pxrb], writes=[xcb])
            for k in range(3):
                S.op('dve', lambda: nc.vector.scalar_tensor_tensor(xc, cin[:, k:k + TT], s.pv(pbase + c * 4 + k), xc, ALU.mult, ALU.add),
                     reads=[cinb], writes=[xcb])
            S.op('dve', lambda: nc.vector.tensor_copy(s.ccar[:, c, 0:3], cin[:, TT:TT + 3]), reads=[cinb], writes=[s.ccarb])
            x16, x16b = s.T16()
            S.op('pool', lambda: nc.gpsimd.tensor_copy(x16, xc), reads=[xcb], writes=[x16b])
            rst[c] = (xc, xcb, x16, x16b, pgr, pgrb)

        def R2(c):
            xc, xcb, x16, x16b, pgr, pgrb = rst.pop(c)
            pr, prb = s.bank('pg', [4, 5])
            pi, pib = s.bank('pg', [4, 5])
            S.op('pe', lambda: nc.tensor.matmul(pr[:], BDv[:, c, 0, :], x16, start=True, stop=True), reads=[BDb, x16b], writes=[prb])
            S.op('pe', lambda: nc.tensor.matmul(pi[:], BDv[:, c, 1, :], x16, start=True, stop=True), reads=[BDb, x16b], writes=[pib])
            ra, rab = s.T32()
            iu, iub = s.T32()
            S.op('act', lambda: nc.scalar.activation(out=ra, in_=pr[:], func=AF.Sigmoid, bias=s.pv(pbase + 40 + c), scale=1.0), reads=[prb], writes=[rab])
            S.op('act', lambda: nc.scalar.activation(out=iu, in_=pi[:], func=AF.Sigmoid, bias=s.pv(pbase + 48 + c), scale=1.0), reads=[pib], writes=[iub])
            S.op('act', lambda: nc.scalar.activation(out=ra, in_=ra, func=AF.Exp, scale=s.cl[:, e, c:c + 1]), reads=[rab, s.parb], writes=[rab])
            S.op('dve', lambda: nc.vector.tensor_tensor(iu, iu, xc, ALU.mult), reads=[iub, xcb], writes=[iub])
            m, mb = s.T32()
            S.op('act', lambda: nc.scalar.activation(out=m, in_=ra, func=AF.Square), reads=[rab], writes=[mb])
            S.op('act', lambda: nc.scalar.activation(out=m, in_=m, func=AF.Sqrt, scale=-1.0, bias=1.0), reads=[mb], writes=[mb])
            S.op('dve', lambda: nc.vector.tensor_tensor(iu, iu, m, ALU.mult), reads=[iub, mb], writes=[iub])
            S.op('dve', lambda: nc.vector.tensor_tensor_scan(m, ra, iu, s.hst[:, c:c + 1], ALU.mult, ALU.add), reads=[rab, iub, s.hstb], writes=[mb])
            S.op('dve', lambda: nc.vector.tensor_copy(s.hst[:, c:c + 1], m[:, TT - 1:TT]), reads=[mb], writes=[s.hstb])
            gg, ggb = s.T32()
            S.op('act', lambda: nc.scalar.activation(out=gg, in_=pgr[:], func=AF.Gelu_apprx_tanh), reads=[pgrb], writes=[ggb])
            S.op('dve', lambda: nc.vector.tensor_tensor(s.big[:, c, :], m, gg, ALU.mult), reads=[mb, ggb], writes=[s.bigb[c]])

        R1(0)
        for c in range(8):
            if c + 1 < 8:
                R1(c + 1)
            R2(c)
        if s.stage < 3:
            return
        Wvs, Wvsb = s.wload(s.wb['w_vs'][e], 128, 2048)
        Wvw, Wvwb = s.wload(s.wb['w_vw'][e], 128, 2048)
        Wgl, Wglb = s.wload(s.wb['w_gl'][e], 128, 384)
        Wvsv = Wvs[:, 0:2048].rearrange("p (k m) -> p k m", k=8)
        Wvwv = Wvw[:, 0:2048].rearrange("p (k m) -> p k m", k=8)
        Wglv = Wgl[:, 0:384].rearrange("p (k m) -> p k m", k=8)
        for j in range(4):
            kt = 4 * i + j
            pv_, pvb = s.bank('pp', [0, 1, 2, 3])
            pg_, pgb_ = s.bank('pp', [0, 1, 2, 3])

            def f():
                for kc in range(8):
                    nc.tensor.matmul(pv_[:, 0:256], s.hT[:, kc, j * 128:(j + 1) * 128], Wvsv[:, kc, :], start=(kc == 0), stop=(kc == 7))
                for kc in range(8):
                    ins = nc.tensor.matmul(pv_[:, 256:512], s.hT[:, kc, j * 128:(j + 1) * 128], Wvwv[:, kc, :], start=(kc == 0), stop=(kc == 7))
                return ins
            S.op('pe', f, reads=[Wvsb, Wvwb] + s.hTb, writes=[pvb])

            def f2():
                for kc in range(8):
                    ins = nc.tensor.matmul(pg_[:, 0:48], s.hT[:, kc, j * 128:(j + 1) * 128], Wglv[:, kc, :], start=(kc == 0), stop=(kc == 7))
                return ins
            S.op('pe', f2, reads=[Wglb] + s.hTb, writes=[pgb_])
            S.op('act', lambda: nc.scalar.copy(s.vsA[:, kt, :, 0:64], pv_[:, 0:256].rearrange("p (g d) -> p g d", g=4)), reads=[pvb], writes=[s.vsAb])
            S.op('act', lambda: nc.scalar.copy(s.vwA[:, kt % 8, :, 0:64], pv_[:, 256:512].rearrange("p (g d) -> p g d", g=4)), reads=[pvb], writes=[s.vwAb])
            S.op('act', lambda: nc.scalar.activation(out=s.gsig[:, j, :], in_=pg_[:, 0:48], func=AF.Sigmoid), reads=[pgb_], writes=[s.gsigb])
        S.op('dve', lambda: nc.vector.tensor_copy(s.kcT[:, :, 0:16], s.kcT[:, :, TT:TT + 16]), reads=[s.kcTb], writes=[s.kcTb])
        S.op('dve', lambda: nc.vector.tensor_copy(s.vcT[:, :, 0:16], s.vcT[:, :, TT:TT + 16]), reads=[s.vcTb], writes=[s.vcTb])
        kvW = []
        for g in range(4):
            W, Wb = s.wload(s.wb['w_kv'][e, g], 128, 2048)
            Wv = W[:, 0:2048].rearrange("p (k t m) -> p k t m", k=8, t=4)
            for ti in range(4):
                pk, pkb = s.bank('pp', [0, 1, 2, 3])

                def f():
                    for kc in range(8):
                        ins = nc.tensor.matmul(pk[0:64, :], Wv[:, kc, ti, :], s.hT[:, kc, :], start=(kc == 0), stop=(kc == 7))
                    return ins
                S.op('pe', f, reads=[Wb] + s.hTb, writes=[pkb])
                if ti < 2:
                    k16, k16b = s.T16()
                    S.op('act', lambda: nc.scalar.copy(k16[0:64, :], pk[0:64, :]), reads=[pkb], writes=[k16b])
                    if ti == 0:
                        s.rope_apply(pk, pkb, k16[0:64, :], k16b, s.ksA[0:64, g, t0:t0 + TT], s.ksAb)
                    else:
                        r0 = (t0 % 1024)
                        s.rope_apply(pk, pkb, k16[0:64, :], k16b, s.kwT[0:64, g, r0:r0 + TT], s.kwTb)
                elif ti == 2:
                    S.op('act', lambda: nc.scalar.copy(s.kcT[:, g, 16:16 + TT], pk[0:64, :]), reads=[pkb], writes=[s.kcTb])
                else:
                    S.op('act', lambda: nc.scalar.copy(s.vcT[:, g, 16:16 + TT], pk[0:64, :]), reads=[pkb], writes=[s.vcTb])
        if s.stage < 4:
            return
        S.dma('sp', s.wcm[:], s.wb['w_cm'][e], writes=[s.wcmb])
        Wcm, Wcmb = s.wcm, s.wcmb
        for kv in range(2):
            srcT, srcTb = (s.kcT, s.kcTb) if kv == 0 else (s.vcT, s.vcTb)
            W1 = []
            for hf in range(2):
                W, Wb = s.wload(s.wb['w_c1'][e, kv, hf], 64, 2048)
                W1.append((W[0:64, 0:2048].rearrange("p (l m) -> p l m", l=16), Wb))
            if i == 0:
                pbi, pbib = s.bank('pp', [0, 1, 2, 3])

                def fb():
                    for l in range(32):
                        Wl, _ = W1[l // 16]
                        c0 = 128 + (kv * 32 + l) * 8
                        ins = nc.tensor.matmul(pbi[:, 0:8], Wl[:, l % 16, :], Wcm[0:64, c0:c0 + 8], start=(l == 0), stop=(l == 31))
                    return ins
                S.op('pe', fb, reads=[W1[0][1], W1[1][1], Wcmb], writes=[pbib])
                S.op('act', lambda: nc.scalar.copy(s.cbias[:, kv:kv + 1], pbi[:, 0:1]), reads=[pbib], writes=[s.cbiasb])
            ph, phb = s.bank('pp', [0, 1, 2, 3])

            def fh():
                for l in range(32):
                    Wl, _ = W1[l // 16]
                    ins = nc.tensor.matmul(ph[:, 0:128], Wl[:, l % 16, :], srcT[:, :, l:l + 16 * 31 + 1:16], start=(l == 0), stop=(l == 31))
                return ins
            S.op('pe', fh, reads=[W1[0][1], W1[1][1], srcTb], writes=[phb])
            hdst, hdstb = (s.hkT, s.hkTb) if kv == 0 else (s.hvT, s.hvTb)
            S.op('act', lambda: nc.scalar.activation(out=hdst[:, :, 32 * i:32 * i + 32], in_=ph[:, 0:128].rearrange("p (g n) -> p g n", g=4),
                                                      func=AF.Gelu_apprx_tanh, bias=s.cbias[:, kv:kv + 1], scale=1.0),
                 reads=[phb, s.cbiasb], writes=[hdstb])
        pkc, pkcb = s.bank('pp', [0, 1, 2, 3])
        S.op('pe', lambda: nc.tensor.matmul(pkc[0:64, :], Wcm[:, 0:64], s.hkT[:].rearrange("p g n -> p (g n)"), start=True, stop=True),
             reads=[Wcmb, s.hkTb], writes=[pkcb])
        S.op('act', lambda: nc.scalar.copy(s.kcmpT[:].rearrange("p g n -> p (g n)"), pkc[0:64, :]), reads=[pkcb], writes=[s.kcmpTb])
        pvc, pvcb = s.bank('pp', [0, 1, 2, 3])

        def fv():
            for g in range(4):
                ins = nc.tensor.matmul(pvc[:, g * 64:(g + 1) * 64], s.hvT[:, g, :], Wcm[:, 64:128], start=True, stop=True, skip_group_check=True)
            return ins
        S.op('pe', fv, reads=[Wcmb, s.hvTb], writes=[pvcb])
        S.op('act', lambda: nc.scalar.copy(s.vcmpA[:, :, 0:64], pvc[:, 0:256].rearrange("p (g d) -> p g d", g=4)), reads=[pvcb], writes=[s.vcmpAb])
        if s.stage < 5:
            return
        for g in range(4):
            W, Wb = s.wload(s.wb['w_q'][e, g], 128, 2048)
            Wv = W[:, 0:2048].rearrange("p (k m) -> p k m", k=8)
            for r in range(4):
                pq, pqb = s.bank('ppa', [0, 1])

                def f():
                    for kc in range(8):
                        ins = nc.tensor.matmul(pq[0:64, :], Wv[:, kc, r * 64:(r + 1) * 64], s.hT[:, kc, :], start=(kc == 0), stop=(kc == 7))
                    return ins
                S.op('pe', f, reads=[Wb] + s.hTb, writes=[pqb])
                S.op('act', lambda: nc.scalar.copy(s.qA[:, r, :], pq[0:64, :]), reads=[pqb], writes=[s.qAb])
                s.rope_apply(pq, pqb, s.qA[:, r, :], s.qAb, s.qrA[0:64, r, :], s.qrAb, bk=('ppa', [0, 1]))
            s.attn_phaseA(g, i)
            if s.stage >= 6:
                s.attn_phaseB(g, i)
        if s.stage < 6:
            return
        for qq in range(4):
            ptr, ptrb = s.ps[7], s.psb[7]
            ptr16 = ptr[:].bitcast(BF16)

            def ft():
                for cc in range(8):
                    ins = nc.tensor.transpose(ptr16[:, cc * 128:(cc + 1) * 128], s.yatt[:, qq, cc * 128:(cc + 1) * 128], s.ident[:])
                return ins
            S.op('pe', ft, reads=[s.yattb[qq], s.constb], writes=[ptrb])
            S.op('act', lambda: nc.scalar.copy(s.big[:, 8:16, qq * 128:(qq + 1) * 128], ptr16[:, 0:1024].rearrange("p (c q) -> p c q", c=8)),
                 reads=[ptrb], writes=s.bigb[8:16])

    def attn_phaseA(s, g, i):
        nc, S = s.nc, s.S
        st = {}

        def A1(qq):
            qs = slice(qq * 128, (qq + 1) * 128)
            psc, pscb = s.bank('sc', [2, 3])

            def f():
                nc.tensor.matmul(psc[:], s.kcmpT[:, g, :], s.qA[:, :, qs], start=True, stop=False)
                return nc.tensor.matmul(psc[:], s.ident[:], s.cmask[:, qs].unsqueeze(1).to_broadcast([128, 4, 128]), start=False, stop=True)
            S.op('pe', f, reads=[s.kcmpTb, s.qAb, s.cmaskb, s.constb], writes=[pscb])
            pc, pcb = s.PT()
            S.op('act', lambda: nc.scalar.activation(out=pc, in_=psc[:], func=AF.Exp, scale=0.125), reads=[pscb], writes=[pcb])
            st[qq] = (pc, pcb)

        def A2(qq):
            qt = 4 * i + qq
            pc, pcb = st[qq]
            poc, pocb = s.bank('poc', [4, 5])

            def f2():
                for r in range(4):
                    ins = nc.tensor.matmul(poc[:, r * 97:(r + 1) * 97], pc[:, r * 128:(r + 1) * 128], s.vcmpA[:, g, 0:97], start=True, stop=True, skip_group_check=True)
                return ins
            S.op('pe', f2, reads=[pcb, s.vcmpAb], writes=[pocb])
            pocv = poc[:, 0:388].rearrange("p (r c) -> p r c", r=4)
            rc, rcb = s.SM()
            S.op('dve', lambda: nc.vector.tensor_scalar(rc, pocv[:, :, 64], 1e-30, None, ALU.max), reads=[pocb], writes=[rcb])
            S.op('dve', lambda: nc.vector.reciprocal(rc, rc), reads=[rcb], writes=[rcb])
            ii = s.rotate('imp', [0, 1])
            imp, impb = s.imp[:, ii, 0:32], s.impb[ii]
            mx8 = s.imp[:, ii, 32:40]
            S.op('dve', lambda: nc.vector.scalar_tensor_tensor(imp, pocv[:, 0, 65:97], rc[:, 0:1], s.bon[:, qq, :], ALU.mult, ALU.add), reads=[pocb, rcb, s.bonb], writes=[impb])
            for r in range(1, 4):
                S.op('dve', lambda: nc.vector.scalar_tensor_tensor(imp, pocv[:, r, 65:97], rc[:, r:r + 1], imp, ALU.mult, ALU.add), reads=[pocb, rcb], writes=[impb])
            S.op('dve', lambda: nc.vector.max(out=mx8, in_=imp), reads=[impb], writes=[impb])
            Mq, Mqb = s.rotate('Mq', [(s.Mq, s.Mqb), (s.Mq2, s.Mq2b)])
            S.op('dve', lambda: nc.vector.tensor_scalar(Mq[:, 64:96], imp, mx8[:, 3:4], MASKV, ALU.is_lt, ALU.mult), reads=[impb], writes=[Mqb])
            st[('m', qq)] = (Mq, Mqb)
            gv = s.gsig[:, qq, g * 12:(g + 1) * 12].rearrange("p (r b) -> p r b", r=4)
            S.op('dve', lambda: nc.vector.tensor_tensor(rc, rc, gv[:, :, 0], ALU.mult), reads=[rcb, s.gsigb], writes=[rcb])
            o1 = s.oc[:, qq, :].rearrange("p (r d) -> p r d", r=4)
            S.op('dve', lambda: nc.vector.tensor_tensor(o1, pocv[:, :, 0:64], rc.unsqueeze(2).to_broadcast([128, 4, 64]), ALU.mult),
                 reads=[pocb, rcb], writes=[s.ocb[qq]])

        def A3(qq):
            qs = slice(qq * 128, (qq + 1) * 128)
            Mq, Mqb = st[('m', qq)]
            ptr, ptrb = s.bank('ptr', [6, 7])
            ptr16 = ptr[:].bitcast(BF16)
            S.op('pe', lambda: nc.tensor.transpose(ptr16[0:96, 0:128], Mq[:, 0:96], s.ident[:]), reads=[Mqb, s.constb], writes=[ptrb])
            S.op('act', lambda: nc.scalar.copy(s.qrA[64:96, :, qs], ptr16[64:96, 0:128].unsqueeze(1).to_broadcast([32, 4, 128])), reads=[ptrb], writes=[s.qrAb])

        for step in range(6):
            if step < 4:
                A1(step)
            if 0 <= step - 1 < 4:
                A2(step - 1)
            if 0 <= step - 2 < 4:
                A3(step - 2)

    def attn_phaseB(s, g, i):
        nc, S = s.nc, s.S
        steps = []
        for qq in range(4):
            qt = 4 * i + qq
            sel = list(range(qt + 1))
            win = [k for k in (qt - 2, qt - 1, qt) if k >= 0]
            for n, kt in enumerate(sel):
                steps.append((qq, 's', kt, n == 0, n == len(sel) - 1))
            for n, kt in enumerate(win):
                steps.append((qq, 'w', kt, n == 0, n == len(win) - 1))

        def score(stp):
            qq, kind, kt, first, last = stp
            qt = 4 * i + qq
            qs = slice(qq * 128, (qq + 1) * 128)
            pss, pssb = s.bank('sc', [2, 3])
            if kind == 's':
                msk = s.mc if kt == qt else None

                def f():
                    ins = nc.tensor.matmul(pss[:], s.ksA[0:96, g, kt * 128:(kt + 1) * 128], s.qrA[0:96, :, qs], start=True, stop=(msk is None))
                    if msk is not None:
                        ins = nc.tensor.matmul(pss[:], s.ident[:], msk[:].unsqueeze(1).to_broadcast([128, 4, 128]), start=False, stop=True)
                    return ins
                S.op('pe', f, reads=[s.ksAb, s.qrAb, s.constb], writes=[pssb])
            else:
                msk = s.mc if kt == qt else (s.mu if kt == qt - 2 else None)
                r0 = (kt % 8) * 128

                def f():
                    ins = nc.tensor.matmul(pss[:], s.kwT[0:64, g, r0:r0 + 128], s.qrA[0:64, :, qs], start=True, stop=(msk is None))
                    if msk is not None:
                        ins = nc.tensor.matmul(pss[:], s.ident[:], msk[:].unsqueeze(1).to_broadcast([128, 4, 128]), start=False, stop=True)
                    return ins
                S.op('pe', f, reads=[s.kwTb, s.qrAb, s.constb], writes=[pssb])
            return pss, pssb

        def finish(stp, pss, pssb):
            qq, kind, kt, first, last = stp
            pt_, ptb_ = s.PT()
            S.op('act', lambda: nc.scalar.activation(out=pt_, in_=pss[:], func=AF.Exp, scale=0.125), reads=[pssb], writes=[ptb_])
            if kind == 's':
                acc, accb = s.ps[4 + qq % 2], s.psb[4 + qq % 2]
                V, Vb = s.vsA[:, kt, g, 0:65], s.vsAb
            else:
                acc, accb = s.ps[6 + qq % 2], s.psb[6 + qq % 2]
                V, Vb = s.vwA[:, kt % 8, g, 0:65], s.vwAb

            def f3():
                for r in range(4):
                    ins = nc.tensor.matmul(acc[:, r * 65:(r + 1) * 65], pt_[:, r * 128:(r + 1) * 128], V,
                                           start=(first and r == 0), stop=(last and r == 3), skip_group_check=True)
                return ins
            S.op('pe', f3, reads=[ptb_, Vb], writes=[accb])
            if kind == 'w' and last:
                combine(qq)

        def combine(qq):
            pos, posb = s.ps[4 + qq % 2], s.psb[4 + qq % 2]
            pow_, powb = s.ps[6 + qq % 2], s.psb[6 + qq % 2]
            posv = pos[:, 0:260].rearrange("p (r c) -> p r c", r=4)
            powv = pow_[:, 0:260].rearrange("p (r c) -> p r c", r=4)
            gv = s.gsig[:, qq, g * 12:(g + 1) * 12].rearrange("p (r b) -> p r b", r=4)
            rs, rsb = s.SM()
            rw, rwb = s.SM()
            S.op('dve', lambda: nc.vector.reciprocal(rs, posv[:, :, 64]), reads=[posb], writes=[rsb])
            S.op('dve', lambda: nc.vector.reciprocal(rw, powv[:, :, 64]), reads=[powb], writes=[rwb])
            S.op('dve', lambda: nc.vector.tensor_tensor(rs, rs, gv[:, :, 1], ALU.mult), reads=[rsb, s.gsigb], writes=[rsb])
            S.op('dve', lambda: nc.vector.tensor_tensor(rw, rw, gv[:, :, 2], ALU.mult), reads=[rwb, s.gsigb], writes=[rwb])
            o1 = s.oc[:, qq, :].rearrange("p (r d) -> p r d", r=4)
            o1b = s.ocb[qq]
            oi2 = 4
            o2 = s.oc[:, oi2, :].rearrange("p (r d) -> p r d", r=4)
            o2b = s.ocb[oi2]
            bc = lambda x: x.unsqueeze(2).to_broadcast([128, 4, 64])
            S.op('dve', lambda: nc.vector.tensor_tensor(o2, posv[:, :, 0:64], bc(rs), ALU.mult), reads=[posb, rsb], writes=[o2b])
            S.op('pool', lambda: nc.gpsimd.tensor_tensor(o1, o1, o2, ALU.add), reads=[o1b, o2b], writes=[o1b])
            S.op('dve', lambda: nc.vector.tensor_tensor(o2, powv[:, :, 0:64], bc(rw), ALU.mult), reads=[powb, rwb], writes=[o2b])
            S.op('pool', lambda: nc.gpsimd.tensor_tensor(s.yatt[:, qq, g * 256:(g + 1) * 256].rearrange("p (r d) -> p r d", r=4), o1, o2, ALU.add),
                 reads=[o1b, o2b], writes=[s.yattb[qq]])

        pending = None
        for stp in steps:
            cur = score(stp)
            if pending is not None:
                finish(*pending)
            pending = (stp,) + cur
        finish(*pending)


_CACHE = {}


def _get_prog(NSEQ, NLAYER, taps=()):
    key = (NSEQ, NLAYER, tuple(taps))
    if key not in _CACHE:
        p = Prog(NSEQ, NLAYER, taps)
        _CACHE[key] = p.build()
    return _CACHE[key]


def make_in_maps(inputs, ncores, nseq):
    hw = host_weights(inputs)
    hc = host_consts()
    x = np.asarray(inputs['x'], dtype=np.float32)
    c = np.asarray(inputs['c'], dtype=np.float32)
    maps = []
    for core in range(ncores):
        b0 = core * nseq
        m = dict(hw)
        m.update(hc)
        m['xT'] = np.ascontiguousarray(x[b0:b0 + nseq].transpose(0, 2, 1))
        cc = c[b0:b0 + nseq]
        m['cT'] = np.ascontiguousarray(cc.reshape(nseq, 8, 128).transpose(2, 1, 0)).reshape(128, 8 * nseq)
        maps.append(m)
    return maps


def kernel(**inputs):
    inputs = {k: np.asarray(v) for k, v in inputs.items()}
    ncores, nseq = 8, 4
    nc = _get_prog(nseq, 4)
    maps = make_in_maps(inputs, ncores, nseq)
    res = run_bass_kernel_spmd(nc, maps, core_ids=list(range(ncores)))
    outs = [np.asarray(r["yT"]).transpose(0, 2, 1) for r in res.results]
    return np.ascontiguousarray(np.concatenate(outs, axis=0).astype(np.float32))
```
